# Optimizing a Trainium2 kernel written in Bass

```python
import jax
import jax.numpy as jnp
from jax import lax
import numpy as np

D_MODEL = 1024
BATCH = 8
SEQ = 2048
DEPTH = 4

HEAD_DIM = 64
ROT_DIM = HEAD_DIM // 4
ROPE_THETA = 500000.0
BLOCK_Q = 128
NORM_EPS = 1e-6
N_EVEN = (DEPTH + 1) // 2
N_ODD = DEPTH // 2

A_HEADS = 8
A_KV_HEADS = 2
IDX_HEADS = 8
IDX_DIM = 32
IDX_ROT = IDX_DIM // 4
TOPK_MAX = 256

B_HEADS = 8
DILATED_PATTERNS = ((128, 1), (512, 4), (2048, 16))

EVEN_SPLITS = (A_HEADS * HEAD_DIM, A_KV_HEADS * HEAD_DIM, A_KV_HEADS * HEAD_DIM,
               IDX_HEADS * IDX_DIM, IDX_DIM, IDX_HEADS,
               B_HEADS * HEAD_DIM, B_HEADS * HEAD_DIM, B_HEADS * HEAD_DIM)
EVEN_IN = sum(EVEN_SPLITS)
EVEN_MIX = (A_HEADS + B_HEADS) * HEAD_DIM

C_HEADS = 8
C_NOPE = 64
C_ROPE = 32
C_V = 64
Q_LORA = 256
KV_LORA = 128

D_RNN = 512
RG_BLOCKS = 8
RG_BW = D_RNN // RG_BLOCKS
CONV_W = 4
RG_C = 8.0

ODD_SPLITS = (Q_LORA, KV_LORA, C_ROPE, D_RNN, D_RNN)
ODD_IN = sum(ODD_SPLITS)
ODD_MIX = C_HEADS * C_V + D_RNN

D_FF = 3584
N_EXPERTS = 8
TOP_K = 2
D_FF_EXPERT = 3584
MOE_BLOCK = 128

kernel_name = 'hybrid_dsa_dilated_mla_rglru_moe'


def rms_norm(x, g):
    xf = x.astype(jnp.float32)
    y = xf * lax.rsqrt(jnp.mean(xf * xf, axis=-1, keepdims=True) + NORM_EPS)
    return (y * g.astype(jnp.float32)).astype(x.dtype)


def apply_rope(x, rot_dim):
    s = x.shape[1]
    half = rot_dim // 2
    inv_freq = 1.0 / (ROPE_THETA ** (jnp.arange(half, dtype=jnp.float32) * (2.0 / rot_dim)))
    ang = jnp.arange(s, dtype=jnp.float32)[:, None] * inv_freq[None, :]
    bshape = (1, s) + (1,) * (x.ndim - 3) + (half,)
    cos = jnp.cos(ang).reshape(bshape)
    sin = jnp.sin(ang).reshape(bshape)
    x1 = x[..., :half].astype(jnp.float32)
    x2 = x[..., half:rot_dim].astype(jnp.float32)
    rot = jnp.concatenate([x1 * cos - x2 * sin, x1 * sin + x2 * cos], axis=-1).astype(x.dtype)
    return jnp.concatenate([rot, x[..., rot_dim:]], axis=-1)


def split_cols(a, sizes):
    return jnp.split(a, [int(c) for c in np.cumsum(sizes)[:-1]], axis=-1)


def swiglu(x, w_gate, w_up, w_down):
    return (jax.nn.silu(x @ w_gate) * (x @ w_up)) @ w_down


def over_query_blocks(fn, b, s):
    out = lax.map(fn, jnp.arange(s // BLOCK_Q))
    return jnp.swapaxes(out, 0, 1).reshape(b, s, out.shape[-1])


def dsa_attention(q, k, v, qi, ki, wi):
    b, s = q.shape[:2]
    topk = min(TOPK_MAX, s // 4)
    group = A_HEADS // A_KV_HEADS
    scale = HEAD_DIM ** -0.5
    gather = jax.vmap(lambda arr, idx: arr[idx])
    key_pos = jnp.arange(s)
    ki32 = ki.astype(jnp.float32)

    def block(i):
        t0 = i * BLOCK_Q
        qpos = t0 + jnp.arange(BLOCK_Q)
        qb = lax.dynamic_slice_in_dim(q, t0, BLOCK_Q, axis=1)
        qib = lax.dynamic_slice_in_dim(qi, t0, BLOCK_Q, axis=1).astype(jnp.float32)
        wib = lax.dynamic_slice_in_dim(wi, t0, BLOCK_Q, axis=1).astype(jnp.float32)
        logit = jnp.einsum('bqhd,bsd->bqhs', qib, ki32) * (IDX_DIM ** -0.5)
        score = jnp.einsum('bqh,bqhs->bqs', wib, jax.nn.relu(logit))
        causal = key_pos[None, :] <= qpos[:, None]
        score = jnp.where(causal[None], score, -jnp.inf)
        _, idx = lax.top_k(score, topk)
        valid = idx <= qpos[None, :, None]
        kg = gather(k, idx)
        vg = gather(v, idx)
        qg = qb.reshape(b, BLOCK_Q, A_KV_HEADS, group, HEAD_DIM)
        sc = jnp.einsum('bqngd,bqknd->bngqk', qg, kg).astype(jnp.float32) * scale
        sc = jnp.where(valid[:, None, None], sc, -jnp.inf)
        p = jax.nn.softmax(sc, axis=-1).astype(v.dtype)
        o = jnp.einsum('bngqk,bqknd->bqngd', p, vg)
        return o.reshape(b, BLOCK_Q, A_HEADS * HEAD_DIM)

    return over_query_blocks(block, b, s)


def dilated_attention(q, k, v):
    b, s, h, dh = q.shape
    scale = dh ** -0.5
    strided = []
    for (w, d) in DILATED_PATTERNS:
        pad = ((0, 0), (w, 0), (0, 0), (0, 0))
        kp = jnp.pad(k, pad).reshape(b, (s + w) // d, d, h, dh)
        vp = jnp.pad(v, pad).reshape(b, (s + w) // d, d, h, dh)
        strided.append((kp, vp))

    def block(i):
        t0 = i * BLOCK_Q
        qb = lax.dynamic_slice_in_dim(q, t0, BLOCK_Q, axis=1)
        outs, lses = [], []
        for (w, d), (kp, vp) in zip(DILATED_PATTERNS, strided):
            n_off = w // d
            nq = BLOCK_Q // d
            band = n_off + nq
            qs = qb.reshape(b, nq, d, h, dh)
            ks = lax.dynamic_slice_in_dim(kp, t0 // d, band, axis=1)
            vs = lax.dynamic_slice_in_dim(vp, t0 // d, band, axis=1)
            sc = jnp.einsum('bmchd,buchd->bhcmu', qs, ks).astype(jnp.float32) * scale
            m_idx = jnp.arange(nq)[:, None]
            u_idx = jnp.arange(band)[None, :]
            j = m_idx + n_off - u_idx
            key_pos = t0 + (u_idx - n_off) * d + jnp.arange(d)[:, None]
            mask = ((j >= 0) & (j <= n_off))[None] & (key_pos >= 0)[:, None, :]
            sc = jnp.where(mask[None, None], sc, -jnp.inf)
            lse = jax.nn.logsumexp(sc, axis=-1)
            p = jnp.exp(sc - lse[..., None]).astype(v.dtype)
            o = jnp.einsum('bhcmu,buchd->bmchd', p, vs).reshape(b, BLOCK_Q, h, dh)
            outs.append(o)
            lses.append(jnp.transpose(lse, (0, 3, 2, 1)).reshape(b, BLOCK_Q, h))
        wts = jax.nn.softmax(jnp.stack(lses, axis=-1), axis=-1).astype(v.dtype)
        o = jnp.einsum('bqhr,rbqhd->bqhd', wts, jnp.stack(outs, axis=0))
        return o.reshape(b, BLOCK_Q, h * dh)

    return over_query_blocks(block, b, s)


def mla_attention(c_q, c_kv, k_rope, q_norm, w_uq, kv_norm, w_ukv):
    b, s = c_q.shape[:2]
    q = (rms_norm(c_q, q_norm) @ w_uq).reshape(b, s, C_HEADS, C_NOPE + C_ROPE)
    q_nope, q_rope = q[..., :C_NOPE], apply_rope(q[..., C_NOPE:], C_ROPE)
    kv = (rms_norm(c_kv, kv_norm) @ w_ukv).reshape(b, s, C_HEADS, C_NOPE + C_V)
    k_nope, v = kv[..., :C_NOPE], kv[..., C_NOPE:]
    k_rope = apply_rope(k_rope, C_ROPE)
    scale = (C_NOPE + C_ROPE) ** -0.5
    key_pos = jnp.arange(s)

    def block(i):
        t0 = i * BLOCK_Q
        qpos = t0 + jnp.arange(BLOCK_Q)
        qn = lax.dynamic_slice_in_dim(q_nope, t0, BLOCK_Q, axis=1)
        qr = lax.dynamic_slice_in_dim(q_rope, t0, BLOCK_Q, axis=1)
        sc = (jnp.einsum('bqhd,bshd->bhqs', qn, k_nope)
              + jnp.einsum('bqhd,bsd->bhqs', qr, k_rope)).astype(jnp.float32) * scale
        causal = key_pos[None, :] <= qpos[:, None]
        sc = jnp.where(causal[None, None], sc, -jnp.inf)
        p = jax.nn.softmax(sc, axis=-1).astype(v.dtype)
        o = jnp.einsum('bhqs,bshd->bqhd', p, v)
        return o.reshape(b, BLOCK_Q, C_HEADS * C_V)

    return over_query_blocks(block, b, s)


def rglru_mixer(xr, gate, conv_w, conv_b, w_a, b_a, w_x, b_x, lam):
    b, s, _ = xr.shape
    xc = lax.conv_general_dilated(xr, conv_w[:, None, :], window_strides=(1,),
                                  padding=((CONV_W - 1, 0),),
                                  dimension_numbers=('NWC', 'WIO', 'NWC'),
                                  feature_group_count=D_RNN)
    xc = (xc + conv_b).astype(jnp.float32)
    xb = xc.reshape(b, s, RG_BLOCKS, RG_BW)
    r = jax.nn.sigmoid(jnp.einsum('bsni,nij->bsnj', xb, w_a.astype(jnp.float32)).reshape(b, s, D_RNN)
                       + b_a.astype(jnp.float32))
    i_g = jax.nn.sigmoid(jnp.einsum('bsni,nij->bsnj', xb, w_x.astype(jnp.float32)).reshape(b, s, D_RNN)
                         + b_x.astype(jnp.float32))
    log_a = -RG_C * jax.nn.softplus(-lam.astype(jnp.float32)) * r
    a = jnp.exp(log_a)
    u = jnp.sqrt(-jnp.expm1(2.0 * log_a)) * (i_g * xc)

    def combine(left, right):
        a_l, b_l = left
        a_r, b_r = right
        return a_l * a_r, a_r * b_l + b_r

    _, h = lax.associative_scan(combine, (a, u), axis=1)
    return (h * jax.nn.gelu(gate.astype(jnp.float32))).astype(xr.dtype)


def moe_swiglu(x, router, w_gate, w_up, w_down):
    b, s, dm = x.shape
    n = b * s
    xf = x.reshape(n, dm)
    logits = (xf @ router).astype(jnp.float32)
    top_val, top_idx = lax.top_k(logits, TOP_K)
    gates = jax.nn.softmax(top_val, axis=-1)
    e_flat = top_idx.reshape(-1)
    g_flat = gates.reshape(-1)
    tok_flat = jnp.repeat(jnp.arange(n, dtype=jnp.int32), TOP_K)
    counts = jnp.bincount(e_flat, length=N_EXPERTS)
    padded = (counts + MOE_BLOCK - 1) // MOE_BLOCK * MOE_BLOCK
    pad_end = jnp.cumsum(padded)
    pad_start = pad_end - padded
    cnt_start = jnp.cumsum(counts) - counts
    order = jnp.argsort(e_flat)
    e_sorted = e_flat[order]
    rank = jnp.arange(n * TOP_K) - cnt_start[e_sorted]
    dest = pad_start[e_sorted] + rank
    n_rows = n * TOP_K + N_EXPERTS * MOE_BLOCK
    row_tok = jnp.full((n_rows,), n, jnp.int32).at[dest].set(tok_flat[order])
    row_gate = jnp.zeros((n_rows,), jnp.float32).at[dest].set(g_flat[order])
    n_blk = n_rows // MOE_BLOCK
    blk_expert = jnp.clip(jnp.searchsorted(pad_end, jnp.arange(n_blk) * MOE_BLOCK, side='right'),
                          0, N_EXPERTS - 1)
    x_rows = jnp.concatenate([xf, jnp.zeros((1, dm), xf.dtype)], axis=0)[row_tok]
    x_rows = x_rows.reshape(n_blk, MOE_BLOCK, dm)

    def expert_block(args):
        xb, e = args
        return swiglu(xb, w_gate[e], w_up[e], w_down[e])

    y_rows = lax.map(expert_block, (x_rows, blk_expert)).reshape(n_rows, dm)
    y_rows = y_rows * row_gate[:, None].astype(y_rows.dtype)
    y = jax.ops.segment_sum(y_rows, row_tok, num_segments=n + 1)[:n]
    return y.reshape(b, s, dm)


def setup_inputs(seed: int = 0) -> dict:
    key = jax.random.key(seed)
    ks = iter(jax.random.split(key, 40))
    res = (2.0 * DEPTH) ** -0.5

    def nrm(shape, fan_in, extra=1.0):
        return jax.random.normal(next(ks), shape, jnp.float32) * (extra * fan_in ** -0.5)

    def gain(shape):
        return 1.0 + 0.05 * jax.random.normal(next(ks), shape, jnp.float32)

    def bias(shape):
        return 0.01 * jax.random.normal(next(ks), shape, jnp.float32)

    x = jax.random.normal(next(ks), (BATCH, SEQ, D_MODEL), jnp.float32)
    u = jax.random.uniform(next(ks), (N_ODD, D_RNN), jnp.float32, minval=0.9, maxval=0.999)
    a0 = u ** (1.0 / RG_C)
    rg_lambda = jnp.log(a0) - jnp.log1p(-a0)
    return {
        'x': x,
        'ev_norm_mix': gain((N_EVEN, D_MODEL)),
        'ev_w_in': nrm((N_EVEN, D_MODEL, EVEN_IN), D_MODEL),
        'ev_w_out': nrm((N_EVEN, EVEN_MIX, D_MODEL), EVEN_MIX, res),
        'ev_norm_ffn': gain((N_EVEN, D_MODEL)),
        'ffn_w_gate': nrm((N_EVEN, D_MODEL, D_FF), D_MODEL),
        'ffn_w_up': nrm((N_EVEN, D_MODEL, D_FF), D_MODEL),
        'ffn_w_down': nrm((N_EVEN, D_FF, D_MODEL), D_FF, res),
        'od_norm_mix': gain((N_ODD, D_MODEL)),
        'od_w_in': nrm((N_ODD, D_MODEL, ODD_IN), D_MODEL),
        'mla_q_norm': gain((N_ODD, Q_LORA)),
        'mla_w_uq': nrm((N_ODD, Q_LORA, C_HEADS * (C_NOPE + C_ROPE)), Q_LORA),
        'mla_kv_norm': gain((N_ODD, KV_LORA)),
        'mla_w_ukv': nrm((N_ODD, KV_LORA, C_HEADS * (C_NOPE + C_V)), KV_LORA),
        'rg_conv_w': nrm((N_ODD, CONV_W, D_RNN), CONV_W),
        'rg_conv_b': bias((N_ODD, D_RNN)),
        'rg_w_a': nrm((N_ODD, RG_BLOCKS, RG_BW, RG_BW), RG_BW),
        'rg_b_a': bias((N_ODD, D_RNN)),
        'rg_w_x': nrm((N_ODD, RG_BLOCKS, RG_BW, RG_BW), RG_BW),
        'rg_b_x': bias((N_ODD, D_RNN)),
        'rg_lambda': rg_lambda,
        'od_w_out': nrm((N_ODD, ODD_MIX, D_MODEL), ODD_MIX, res),
        'od_norm_ffn': gain((N_ODD, D_MODEL)),
        'moe_router': nrm((N_ODD, D_MODEL, N_EXPERTS), D_MODEL),
        'moe_w_gate': nrm((N_ODD, N_EXPERTS, D_MODEL, D_FF_EXPERT), D_MODEL),
        'moe_w_up': nrm((N_ODD, N_EXPERTS, D_MODEL, D_FF_EXPERT), D_MODEL),
        'moe_w_down': nrm((N_ODD, N_EXPERTS, D_FF_EXPERT, D_MODEL), D_FF_EXPERT, res),
        'final_norm': gain((D_MODEL,)),
    }


def reference(x, ev_norm_mix, ev_w_in, ev_w_out, ev_norm_ffn, ffn_w_gate, ffn_w_up, ffn_w_down,
              od_norm_mix, od_w_in, mla_q_norm, mla_w_uq, mla_kv_norm, mla_w_ukv,
              rg_conv_w, rg_conv_b, rg_w_a, rg_b_a, rg_w_x, rg_b_x, rg_lambda,
              od_w_out, od_norm_ffn, moe_router, moe_w_gate, moe_w_up, moe_w_down, final_norm):
    b, s, _ = x.shape
    for layer in range(DEPTH):
        i = layer // 2
        if layer % 2 == 0:
            h = rms_norm(x, ev_norm_mix[i])
            qa, ka, va, qi, ki, wi, qb, kb, vb = split_cols(h @ ev_w_in[i], EVEN_SPLITS)
            qa = apply_rope(qa.reshape(b, s, A_HEADS, HEAD_DIM), ROT_DIM)
            ka = apply_rope(ka.reshape(b, s, A_KV_HEADS, HEAD_DIM), ROT_DIM)
            va = va.reshape(b, s, A_KV_HEADS, HEAD_DIM)
            qi = apply_rope(qi.reshape(b, s, IDX_HEADS, IDX_DIM), IDX_ROT)
            ki = apply_rope(ki, IDX_ROT)
            wi = wi * (IDX_HEADS ** -0.5)
            out_a = dsa_attention(qa, ka, va, qi, ki, wi)
            qb = apply_rope(qb.reshape(b, s, B_HEADS, HEAD_DIM), ROT_DIM)
            kb = apply_rope(kb.reshape(b, s, B_HEADS, HEAD_DIM), ROT_DIM)
            vb = vb.reshape(b, s, B_HEADS, HEAD_DIM)
            out_b = dilated_attention(qb, kb, vb)
            x = x + jnp.concatenate([out_a, out_b], axis=-1) @ ev_w_out[i]
            x = x + swiglu(rms_norm(x, ev_norm_ffn[i]), ffn_w_gate[i], ffn_w_up[i], ffn_w_down[i])
        else:
            h = rms_norm(x, od_norm_mix[i])
            c_q, c_kv, k_rope, x_rnn, g_rnn = split_cols(h @ od_w_in[i], ODD_SPLITS)
            out_c = mla_attention(c_q, c_kv, k_rope, mla_q_norm[i], mla_w_uq[i],
                                  mla_kv_norm[i], mla_w_ukv[i])
            out_d = rglru_mixer(x_rnn, g_rnn, rg_conv_w[i], rg_conv_b[i], rg_w_a[i], rg_b_a[i],
                                rg_w_x[i], rg_b_x[i], rg_lambda[i])
            x = x + jnp.concatenate([out_c, out_d], axis=-1) @ od_w_out[i]
            x = x + moe_swiglu(rms_norm(x, od_norm_ffn[i]), moe_router[i], moe_w_gate[i],
                               moe_w_up[i], moe_w_down[i])
    return rms_norm(x, final_norm)
```

```python
import numpy as np
import concourse.bass as bass
import concourse.mybir as mybir
from contextlib import ExitStack

F32 = mybir.dt.float32
BF16 = mybir.dt.bfloat16
I32 = mybir.dt.int32
U32 = mybir.dt.uint32
AF = mybir.ActivationFunctionType
ALU = mybir.AluOpType
AX = mybir.AxisListType

ENGS = ("pe", "act", "dve", "pool", "sp")


class Rec:
    def __init__(self, nc):
        self.nc = nc
        self.stack = ExitStack()
        self.sems = {e: self.stack.enter_context(nc.semaphore("e_" + e)) for e in ENGS}
        self.base = {e: 0 for e in ENGS}
        self.dma_sems = {}
        self.dma_touched = set()
        self.prev_barrier = None
        self.final_tokens = []
        self.n_instr = 0
        self._reset()

    def _reset(self):
        self.ops = {e: [] for e in ENGS}
        self.lastw = {}
        self.readers = {}

    def sb(self, name, shape, dt, stack=None):
        self.uid = getattr(self, "uid", 0) + 1
        return (stack or self.stack).enter_context(self.nc.sbuf_tensor("%s_%d" % (name, self.uid), list(shape), dt))

    def ps(self, name, shape, dt=F32, stack=None):
        self.uid = getattr(self, "uid", 0) + 1
        return (stack or self.stack).enter_context(self.nc.psum_tensor("%s_%d" % (name, self.uid), list(shape), dt))

    def _deps(self, reads, writes):
        deps = set()
        for k in reads:
            t = self.lastw.get(k)
            if t is not None:
                deps.add(t)
        for k in writes:
            t = self.lastw.get(k)
            if t is not None:
                deps.add(t)
            for r in self.readers.get(k, ()):
                deps.add(r)
        return deps

    def _commit(self, tok, reads, writes):
        for k in reads:
            self.readers.setdefault(k, []).append(tok)
        for k in writes:
            self.lastw[k] = tok
            self.readers[k] = []

    PSUM_KEYS = {"ptrn", "Ap", "Aptr", "Aptrb", "pl", "pmT", "pS", "pO", "pY", "pp", "Bptr", "pG", "pU", "pc", "Cptr",
                 "pq", "pkv", "Cptr2", "pxr", "pgt", "pa", "px"}

    def op(self, eng, fn, reads=(), writes=()):
        ex = [k for k in reads if (k[0] if isinstance(k, tuple) else k) in self.PSUM_KEYS]
        if ex:
            reads = [k for k in reads if k not in ex]
            writes = list(writes) + ex
        deps = self._deps(reads, writes)
        idx = len(self.ops[eng])
        tok = ("E", eng, idx)
        self.ops[eng].append(dict(fn=fn, deps=deps, signal=False, dma=None))
        self._commit(tok, reads, writes)
        return tok

    def dma(self, eng, fn, sem, reads=(), writes=(), final=False):
        deps = self._deps(reads, writes)
        if sem not in self.dma_sems:
            h = self.stack.enter_context(self.nc.semaphore("d_" + sem))
            self.dma_sems[sem] = [h, 0]
        ent = self.dma_sems[sem]
        ent[1] += 16
        self.dma_touched.add(sem)
        tok = ("D", sem, ent[1])
        if ent[1] > 16:
            deps.add(("D", sem, ent[1] - 16))
        self.ops[eng].append(dict(fn=fn, deps=deps, signal=False, dma=sem))
        self._commit(tok, reads, writes)
        if final:
            self.final_tokens.append(tok)
        return tok

    def emit_phase(self, last=False):
        nc = self.nc
        ops = self.ops
        sems = self.sems
        dma_sems = self.dma_sems
        for e in ENGS:
            for o in ops[e]:
                for d in o["deps"]:
                    if d[0] == "E":
                        if d[1] == "pe" and e == "pe":
                            continue
                        ops[d[1]][d[2]]["signal"] = True
            for o in reversed(ops[e]):
                if o["dma"] is None:
                    o["signal"] = True
                    break
        sigval = {}
        for e in ENGS:
            c = self.base[e]
            v = []
            for o in ops[e]:
                if o["signal"]:
                    c += 1
                v.append(c)
            sigval[e] = v
        new_base = {e: (sigval[e][-1] if sigval[e] else self.base[e]) for e in ENGS}
        prev = self.prev_barrier
        final_tokens = self.final_tokens if last else []

        def run(e, engobj):
            waited = {}
            if prev is not None:
                for e2, val in prev[0].items():
                    if e2 != e and val > 0:
                        engobj.wait_ge(sems[e2], val)
                        waited[("E", e2)] = val
                for s, val in prev[1].items():
                    engobj.wait_ge(dma_sems[s][0], val)
                    waited[("D", s)] = val
            for o in ops[e]:
                need = {}
                for d in o["deps"]:
                    if d[0] == "E":
                        if d[1] == "pe" and e == "pe":
                            continue
                        key = ("E", d[1])
                        val = sigval[d[1]][d[2]]
                    else:
                        key = ("D", d[1])
                        val = d[2]
                    if val > need.get(key, 0):
                        need[key] = val
                for key, val in need.items():
                    if waited.get(key, 0) >= val:
                        continue
                    waited[key] = val
                    if key[0] == "E":
                        engobj.wait_ge(sems[key[1]], val)
                    else:
                        engobj.wait_ge(dma_sems[key[1]][0], val)
                ins = o["fn"](engobj)
                self.n_instr += 1
                if o["dma"] is not None:
                    ins.then_inc(dma_sems[o["dma"]][0], 16)
                elif o["signal"]:
                    ins.then_inc(sems[e], 1)
            if last and e == "sp":
                for t in final_tokens:
                    engobj.wait_ge(dma_sems[t[1]][0], t[2])

        with nc.Block() as block:
            @block.tensor
            def _(eng):
                run("pe", eng)

            @block.scalar
            def _(eng):
                run("act", eng)

            @block.vector
            def _(eng):
                run("dve", eng)

            @block.gpsimd
            def _(eng):
                run("pool", eng)

            @block.sync
            def _(eng):
                run("sp", eng)

        self.base = new_base
        self.prev_barrier = (dict(new_base), {s: dma_sems[s][1] for s in self.dma_touched})
        self.dma_touched = set()
        self._reset()

    def close(self):
        self.stack.close()


S = 2048
D = 1024
NT = 16
DFF = 3584
NSLAB = 7
EPS = 1e-6
THETA = 500000.0
NEG = -1.0e30
import math

IN_SHAPES = {
    'ev_norm_mix': (2, 1024), 'ev_w_in': (2, 1024, 2600), 'ev_w_out': (2, 1024, 1024), 'ev_norm_ffn': (2, 1024),
    'ffn_w_gate': (2, 1024, 3584), 'ffn_w_up': (2, 1024, 3584), 'ffn_w_down': (2, 3584, 1024),
    'od_norm_mix': (2, 1024), 'od_w_in': (2, 1024, 1440), 'mla_q_norm': (2, 256), 'mla_w_uq': (2, 256, 768),
    'mla_kv_norm': (2, 128), 'mla_w_ukv': (2, 128, 1024), 'rg_conv_w': (2, 4, 512), 'rg_conv_b': (2, 512),
    'rg_w_a': (2, 8, 64, 64), 'rg_b_a': (2, 512), 'rg_w_x': (2, 8, 64, 64), 'rg_b_x': (2, 512), 'rg_lambda': (2, 512),
    'od_w_out': (2, 1024, 1024), 'od_norm_ffn': (2, 1024), 'moe_router': (2, 1024, 8),
    'moe_w_gate': (2, 8, 1024, 3584), 'moe_w_up': (2, 8, 1024, 3584), 'moe_w_down': (2, 8, 3584, 1024),
    'final_norm': (1, 1024),
}


class K:
    pass


def build(layers=(0, 1, 2, 3), do_final=True, stop=None, n_experts=8):
    nc = bass.Bass("TRN2", target_bir_lowering=False)
    g = K()
    g.nc = nc
    g.W = {}
    g.x_in = nc.dram_tensor("x", [S, D], F32, kind="ExternalInput").ap()
    for name, shp in IN_SHAPES.items():
        g.W[name] = nc.dram_tensor(name, list(shp), F32, kind="ExternalInput").ap()
    g.out = nc.dram_tensor("out", [S, D], F32, kind="ExternalOutput").ap()
    R = Rec(nc)
    g.R = R
    g.X = R.sb("X", [128, NT, D], F32)
    g.HT = R.sb("HT", [128, 8, S], BF16)
    g.ident = R.sb("ident", [128, 128], BF16)
    g.gbc = R.sb("gbc", [128, D], F32)
    g.ss = R.sb("ss", [128, NT], F32)
    g.rstd = R.sb("rstd", [128, NT], F32)
    g.TB = R.sb("TB", [128, S], BF16)
    g.TC = R.sb("TC", [128, 512], BF16)
    g.NTRI = R.sb("NTRI", [128, 128], F32)
    g.rope = {}
    for rot in (16, 8, 32):
        g.rope[rot] = (R.sb("cos%d" % rot, [128, NT, rot // 2], F32), R.sb("sin%d" % rot, [128, NT, rot // 2], F32))
    g.n_experts = n_experts

    first = layers[0] if len(layers) else None
    first_gain = None
    if first is not None:
        first_gain = (g.W['ev_norm_mix'] if first % 2 == 0 else g.W['od_norm_mix'])[first // 2:first // 2 + 1, :]
    phase_init(g, first_gain)
    merged_final = False
    pre_normed = True
    for li, L in enumerate(layers):
        i = L // 2
        is_last = (L == layers[-1]) and stop is None
        nxt = layers[li + 1] if li + 1 < len(layers) else None
        nxt_gain = None
        if nxt is not None and stop is None:
            nxt_gain = (g.W['ev_norm_mix'] if nxt % 2 == 0 else g.W['od_norm_mix'])[nxt // 2:nxt // 2 + 1, :]
        ntail = (lambda st, ng=nxt_gain: phase_norm(g, ng, st=st, reuse=True)) if nxt_gain is not None else None
        if L % 2 == 0:
            if not pre_normed:
                phase_norm(g, g.W['ev_norm_mix'][i:i + 1, :])
            phase_A(g, i)
            phase_B(g, i)
            if stop == "mix":
                break
            phase_ffn(g, g.W['ffn_w_gate'][i], g.W['ffn_w_up'][i], g.W['ffn_w_down'][i], None, norm_gain=g.W['ev_norm_ffn'][i:i + 1, :], tail=ntail)
            pre_normed = ntail is not None
        else:
            phase_C(g, i, norm_gain=(None if pre_normed else g.W['od_norm_mix'][i:i + 1, :]))
            phase_D(g, i)
            if stop == "mix":
                break
            tail = (lambda st: phase_final(g, do_final, st=st)) if is_last else None
            phase_moe(g, i, norm_gain=g.W['od_norm_ffn'][i:i + 1, :], tail=tail, ntail=ntail)
            merged_final = merged_final or is_last
            pre_normed = ntail is not None
    if not merged_final:
        phase_final(g, do_final)
    R.close()
    return nc


def phase_init(g, first_gain=None):
    R = g.R
    X = g.X
    xin = g.x_in.rearrange("(t p) d -> p t d", p=128)
    for t in range(NT):
        R.dma("sp" if t % 2 == 0 else "act", lambda e, t=t: e.dma_start(out=X[:, t, :], in_=xin[:, t, :]),
              "xin%d" % (t % 4), writes=[("X", t, 0), ("X", t, 1)])
    with ExitStack() as st:
        Di = R.sb("Di", [128, S], I32, st)
        Df = R.sb("Df", [128, S], F32, st)
        Ai = R.sb("Ai", [128, S], I32, st)
        m0 = R.sb("m0", [128, S], F32, st)
        m1 = R.sb("m1", [128, S], F32, st)
        m2 = R.sb("m2", [128, S], F32, st)
        ident = g.ident
        R.op("pool", lambda e: e.memset(ident[:], 0.0), writes=["ident"])
        R.op("pool", lambda e: e.affine_select(out=ident[:], in_=ident[:], pattern=[[-1, 128]], compare_op=ALU.not_equal,
                                               fill=1.0, base=0, channel_multiplier=1), reads=["ident"], writes=["ident"])
        R.op("pool", lambda e: e.iota(Di[:], pattern=[[1, S]], base=0, channel_multiplier=-1), writes=["Di"])
        R.op("dve", lambda e: e.tensor_copy(out=Df[:], in_=Di[:]), reads=["Di"], writes=["Df"])
        R.op("dve", lambda e: e.tensor_scalar(out=m0[:], in0=Df[:], scalar1=0.0, scalar2=None, op0=ALU.is_ge), reads=["Df"], writes=["m0"])
        TC, NTRI, TB = g.TC, g.NTRI, g.TB
        R.op("dve", lambda e: e.tensor_copy(out=TC[:], in_=m0[:, 0:512]), reads=["m0"], writes=["TC"])
        R.op("dve", lambda e: e.tensor_scalar(out=NTRI[:], in0=Df[:, 0:128], scalar1=0.0, scalar2=2.0 * NEG, op0=ALU.is_gt, op1=ALU.mult),
             reads=["Df"], writes=["NTRI"])
        R.op("dve", lambda e: e.scalar_tensor_tensor(out=m1[:], in0=Df[:], scalar=128.0, in1=m0[:], op0=ALU.is_le, op1=ALU.mult),
             reads=["Df", "m0"], writes=["m1"])
        R.op("dve", lambda e: e.tensor_single_scalar(out=Ai[:], in_=Di[:], scalar=3, op=ALU.bitwise_and), reads=["Di"], writes=["Ai"])
        R.op("dve", lambda e: e.tensor_copy(out=m2[:], in_=Ai[:]), reads=["Ai"], writes=["m2"])
        R.op("dve", lambda e: e.scalar_tensor_tensor(out=m2[:], in0=m2[:], scalar=0.0, in1=m0[:], op0=ALU.is_equal, op1=ALU.mult),
             reads=["m2", "m0"], writes=["m2"])
        R.op("dve", lambda e: e.scalar_tensor_tensor(out=m2[:], in0=Df[:], scalar=512.0, in1=m2[:], op0=ALU.is_le, op1=ALU.mult),
             reads=["m2", "Df"], writes=["m2"])
        R.op("dve", lambda e: e.tensor_tensor(out=m1[:], in0=m1[:], in1=m2[:], op=ALU.add), reads=["m1", "m2"], writes=["m1"])
        R.op("dve", lambda e: e.tensor_single_scalar(out=Ai[:], in_=Di[:], scalar=15, op=ALU.bitwise_and), reads=["Di"], writes=["Ai"])
        R.op("dve", lambda e: e.tensor_copy(out=m2[:], in_=Ai[:]), reads=["Ai"], writes=["m2"])
        R.op("dve", lambda e: e.scalar_tensor_tensor(out=m2[:], in0=m2[:], scalar=0.0, in1=m0[:], op0=ALU.is_equal, op1=ALU.mult),
             reads=["m2", "m0"], writes=["m2"])
        R.op("dve", lambda e: e.tensor_tensor(out=TB[:], in0=m1[:], in1=m2[:], op=ALU.add), reads=["m1", "m2"], writes=["TB"])
        posi = R.sb("posi", [128, NT], I32, st)
        posf = R.sb("posf", [128, NT], F32, st)
        R.op("pool", lambda e: e.iota(posi[:], pattern=[[128, NT]], base=0, channel_multiplier=1), writes=["posi"])
        R.op("dve", lambda e: e.tensor_copy(out=posf[:], in_=posi[:]), reads=["posi"], writes=["posf"])
        for rot in (16, 8, 32):
            nf = rot // 2
            fi = R.sb("fi%d" % rot, [128, nf], I32, st)
            ff = R.sb("ff%d" % rot, [128, nf], F32, st)
            ang = R.sb("ang%d" % rot, [128, NT, nf], F32, st)
            ang2 = R.sb("ang2%d" % rot, [128, NT, nf], F32, st)
            ki = R.sb("ki%d" % rot, [128, NT, nf], I32, st)
            kf = R.sb("kf%d" % rot, [128, NT, nf], F32, st)
            cs, sn = g.rope[rot]
            k0 = "r%d" % rot
            R.op("pool", lambda e, fi=fi, nf=nf: e.iota(fi[:], pattern=[[1, nf]], base=0, channel_multiplier=0), writes=[k0 + "fi"])
            R.op("dve", lambda e, fi=fi, ff=ff: e.tensor_copy(out=ff[:], in_=fi[:]), reads=[k0 + "fi"], writes=[k0 + "ff"])
            R.op("act", lambda e, ff=ff, rot=rot: e.activation(out=ff[:], in_=ff[:], func=AF.Exp, scale=-math.log(THETA) * 2.0 / rot),
                 reads=[k0 + "ff"], writes=[k0 + "ff"])
            R.op("dve", lambda e, ang=ang, ff=ff, nf=nf: e.tensor_tensor(
                out=ang[:], in0=posf[:].unsqueeze(2).to_broadcast([128, NT, nf]),
                in1=ff[:].unsqueeze(1).to_broadcast([128, NT, nf]), op=ALU.mult), reads=["posf", k0 + "ff"], writes=[k0 + "ang"])
            for which, dst, shift in (("s", sn, 0.0), ("c", cs, math.pi / 2)):
                kk = k0 + which
                R.op("dve", lambda e, ang=ang, ang2=ang2, shift=shift: e.tensor_scalar(
                    out=ang2[:], in0=ang[:], scalar1=shift, scalar2=None, op0=ALU.add), reads=[k0 + "ang"], writes=[k0 + "ang2"])
                R.op("dve", lambda e, ang2=ang2, ki=ki: e.tensor_scalar(
                    out=ki[:], in0=ang2[:], scalar1=1.0 / (2 * math.pi), scalar2=None, op0=ALU.mult), reads=[k0 + "ang2"], writes=[k0 + "ki"])
                R.op("dve", lambda e, kf=kf, ki=ki: e.tensor_copy(out=kf[:], in_=ki[:]), reads=[k0 + "ki"], writes=[k0 + "kf"])
                R.op("dve", lambda e, kf=kf, ang2=ang2: e.scalar_tensor_tensor(
                    out=ang2[:], in0=kf[:], scalar=-2 * math.pi, in1=ang2[:], op0=ALU.mult, op1=ALU.add),
                    reads=[k0 + "kf", k0 + "ang2"], writes=[k0 + "ang2"])
                R.op("dve", lambda e, ang2=ang2: e.tensor_scalar(
                    out=ang2[:], in0=ang2[:], scalar1=3.14159, scalar2=-3.14159, op0=ALU.min, op1=ALU.max),
                    reads=[k0 + "ang2"], writes=[k0 + "ang2"])
                R.op("act", lambda e, ang2=ang2, dst=dst: e.activation(out=dst[:], in_=ang2[:], func=AF.Sin),
                     reads=[k0 + "ang2"], writes=[kk + "tab"])
        if first_gain is not None:
            phase_norm(g, first_gain, st=st)
        R.emit_phase()


def phase_norm(g, gain_row, emit=True, st=None, reuse=False):
    R = g.R
    X, HT, gbc, ss, rstd, ident = g.X, g.HT, g.gbc, g.ss, g.rstd, g.ident
    own = ExitStack() if st is None else None
    if st is not None:
        emit = False
    with (own if own is not None else ExitStack()) as st_:
        st = st_ if own is not None else st
        if reuse:
            junk, hb, ptr = g._norm_bufs
        else:
            junk = R.sb("junk", [128, D], BF16, st)
            hb = [R.sb("hb%d" % b, [128, D], BF16, st) for b in range(2)]
            ptr = [R.ps("ptrn%d" % b, [128, 8, 128], BF16, st) for b in range(2)]
            g._norm_bufs = (junk, hb, ptr)
        R.dma("sp", lambda e: e.dma_start(out=gbc[:], in_=gain_row.to_broadcast([128, D])), "gbc", writes=["gbc"])
        for t in range(NT):
            R.op("act", lambda e, t=t: e.activation(out=junk[:], in_=X[:, t, :], func=AF.Square, accum_out=ss[:, t:t + 1]),
                 reads=[("X", t, 0), ("X", t, 1)], writes=["junk", ("ss", t)])
        allss = [("ss", t) for t in range(NT)]
        R.op("dve", lambda e: e.tensor_scalar(out=rstd[:], in0=ss[:], scalar1=1.0 / D, scalar2=EPS, op0=ALU.mult, op1=ALU.add),
             reads=allss, writes=["rstd"])
        R.op("act", lambda e: e.activation(out=rstd[:], in_=rstd[:], func=AF.Sqrt), reads=["rstd"], writes=["rstd"])
        R.op("dve", lambda e: e.reciprocal(out=rstd[:], in_=rstd[:]), reads=["rstd"], writes=["rstd"])
        for t in range(NT):
            b = t % 2
            R.op("dve", lambda e, t=t, b=b: e.scalar_tensor_tensor(out=hb[b][:], in0=X[:, t, :], scalar=rstd[:, t:t + 1], in1=gbc[:],
                                                                   op0=ALU.mult, op1=ALU.mult),
                 reads=[("X", t, 0), ("X", t, 1), "rstd", "gbc"], writes=[("hb", b)])
            for c in range(8):
                R.op("pe", lambda e, b=b, c=c: e.transpose(out=ptr[b][:, c, :], in_=hb[b][:, c * 128:(c + 1) * 128], identity=ident[:]),
                     reads=[("hb", b), "ident"], writes=[("ptrn", b)])
            R.op("act", lambda e, t=t, b=b: e.activation(out=HT[:, :, t * 128:(t + 1) * 128], in_=ptr[b][:], func=AF.Copy),
                 reads=[("ptrn", b)], writes=[("HT", t)])
        if emit:
            R.emit_phase()


def rope_tok(g, src, dst, nh, off, rot, t, tmp, rkeys, wkeys):
    R = g.R
    half = rot // 2
    cs, sn = g.rope[rot]
    cb = cs[:, t, :].unsqueeze(1).to_broadcast([128, nh, half])
    sb_ = sn[:, t, :].unsqueeze(1).to_broadcast([128, nh, half])
    x1 = src[:, :, off:off + half]
    x2 = src[:, :, off + half:off + rot]
    t1 = tmp[0][:, 0:nh * half].rearrange("p (h f) -> p h f", h=nh)
    t2 = tmp[1][:, 0:nh * half].rearrange("p (h f) -> p h f", h=nh)
    k1, k2 = ("rt", 0), ("rt", 1)
    R.op("dve", lambda e: e.tensor_tensor(out=t1, in0=x1, in1=cb, op=ALU.mult), reads=rkeys, writes=[k1])
    R.op("dve", lambda e: e.tensor_tensor(out=t2, in0=x2, in1=sb_, op=ALU.mult), reads=rkeys, writes=[k2])
    R.op("dve", lambda e: e.tensor_tensor(out=dst[:, :, off:off + half], in0=t1, in1=t2, op=ALU.subtract), reads=[k1, k2], writes=wkeys)
    R.op("dve", lambda e: e.tensor_tensor(out=t1, in0=x1, in1=sb_, op=ALU.mult), reads=rkeys, writes=[k1])
    R.op("dve", lambda e: e.tensor_tensor(out=t2, in0=x2, in1=cb, op=ALU.mult), reads=rkeys, writes=[k2])
    R.op("dve", lambda e: e.tensor_tensor(out=dst[:, :, off + half:off + rot], in0=t1, in1=t2, op=ALU.add), reads=[k1, k2], writes=wkeys)


def wdma(g, dst, src, sem, key):
    g.R.dma("pool", lambda e: e.dma_start(out=dst, in_=src), sem, writes=[key])


def outproj_add(g, lhs_fn, nk, Wo, wkey, t, pY, mixkeys, pykey="pY"):
    R = g.R
    X = g.X
    for half in range(2):
        b = (t * 2 + half) % len(pY)
        for k in range(nk):
            R.op("pe", lambda e, k=k, b=b, half=half: e.matmul(pY[b][:], lhsT=lhs_fn(k), rhs=Wo[:, k, half * 512:(half + 1) * 512],
                                                            start=(k == 0), stop=(k == nk - 1)),
                 reads=mixkeys + [wkey], writes=[(pykey, b)])
        R.op("dve", lambda e, b=b, half=half: e.tensor_tensor(out=X[:, t, half * 512:(half + 1) * 512], in0=pY[b][:],
                                                              in1=X[:, t, half * 512:(half + 1) * 512], op=ALU.add),
             reads=[(pykey, b), ("X", t, half)], writes=[("X", t, half)])


def attn_pair(g, nm, q_fn, k_fn, v_fn, in_keys, scale, mode, mixp, mixkey, pS, pO, E, P, rden, after_chunk=None, mask_eng="pool", filler=None, own_outproj=None):
    R = g.R
    NB = len(pS)
    LOOK = NB - 1
    steps = []
    for c in range(4):
        for hh in range(2):
            nj = 4 * c + 4
            for j in range(nj):
                steps.append((c, hh, j, nj))
    deferred = []
    info = {}

    def emit_qk(s):
        c, hh, j, nj = steps[s]
        r = max(0, j - 4 * c)
        N = 512 - 128 * r
        q0 = (4 * c + r) * 128
        X0 = q0 - 128 * j
        b = s % NB
        R.op("pe", lambda e: e.matmul(pS[b][:, 0:N], lhsT=k_fn(hh, j), rhs=q_fn(hh, q0, N), start=True, stop=True),
             reads=in_keys, writes=[("pS", b)])
        R.op("act", lambda e: e.activation(out=E[b][:, 0:N], in_=pS[b][:, 0:N], func=AF.Exp, scale=scale),
             reads=[("pS", b)], writes=[("E", b)])
        if mode == "B" or X0 == 0:
            tab = g.TB[:, X0:X0 + N] if mode == "B" else g.TC[:, 0:N]
            eng = mask_eng if isinstance(mask_eng, str) else mask_eng[s % len(mask_eng)]
            R.op(eng, lambda e: e.tensor_tensor(out=P[b][:, 0:N], in0=E[b][:, 0:N], in1=tab, op=ALU.mult),
                 reads=[("E", b)], writes=[("P", b)])
            info[s] = (P[b], ("P", b), r, N)
        else:
            info[s] = (E[b], ("E", b), r, N)

    def emit_pv(s):
        c, hh, j, nj = steps[s]
        src, skey, r, N = info.pop(s)
        bo = (c * 2 + hh) % len(pO)
        R.op("pe", lambda e: e.matmul(pO[bo][:, 128 * r:512], lhsT=v_fn(hh, j), rhs=src[:, 0:N], start=(j == 0), stop=(j == nj - 1)),
             reads=[skey] + in_keys, writes=[("pO", bo)])
        if j == nj - 1:
            R.op("act", lambda e: e.activation(out=rden[64:128, :], in_=pO[bo][64:128, :], func=AF.Ln), reads=[("pO", bo)], writes=["rden"])
            R.op("act", lambda e: e.activation(out=rden[64:128, :], in_=rden[64:128, :], func=AF.Exp, scale=-1.0), reads=["rden"], writes=["rden"])
            R.op("dve", lambda e: e.tensor_tensor(out=mixp[hh * 64:(hh + 1) * 64, c * 512:(c + 1) * 512],
                                                  in0=pO[bo][0:64, :], in1=rden[64:128, :], op=ALU.mult),
                 reads=[("pO", bo), "rden"], writes=[(mixkey, c, hh)])
            if hh == 1 and after_chunk is not None:
                deferred.append((s + 4, lambda c=c: after_chunk(c)))
            if hh == 1 and own_outproj is not None:
                own.append([s + 4, own_outproj(c)])

    n = len(steps)
    own = []
    for s in range(n + LOOK):
        if s < n:
            emit_qk(s)
            if filler is not None:
                filler()
        if s - LOOK >= 0:
            emit_pv(s - LOOK)
            while deferred and deferred[0][0] <= s - LOOK:
                deferred.pop(0)[1]()
            if own and own[0][0] <= s - LOOK:
                if next(own[0][1], "end") == "end":
                    own.pop(0)
    while deferred:
        deferred.pop(0)[1]()
    for _, gen_ in own:
        for _ in gen_:
            pass


def phase_A(g, i):
    R = g.R
    HT, X, ident = g.HT, g.X, g.ident
    w_in = g.W['ev_w_in'][i]
    w_out = g.W['ev_w_out'][i]
    wi_scale = (8 ** -0.5) * (32 ** -0.5)
    with ExitStack() as st:
        qT = R.sb("A_qT", [128, NT, 4, 128], BF16, st)
        kT = R.sb("A_kT", [128, 2, S], BF16, st)
        vaug = R.sb("A_vaug", [128, NT, 3, 64], BF16, st)
        qiT = R.sb("A_qiT", [128, 3, S], BF16, st)
        kiT = R.sb("A_kiT", [128, S], BF16, st)
        wiS = R.sb("A_wiS", [128, NT, 8], F32, st)
        WoA = R.sb("A_Wo", [128, 4, D], BF16, st)
        with ExitStack() as st2:
            WA = R.sb("A_W", [128, 8, 1064], BF16, st2)
            stq = [R.sb("A_stq%d" % b, [128, 512], BF16, st2) for b in range(2)]
            stk = [R.sb("A_stk%d" % b, [128, 128], BF16, st2) for b in range(2)]
            sti = [R.sb("A_sti%d" % b, [128, 416], BF16, st2) for b in range(2)]
            tmp = [R.sb("A_rt%d" % b, [128, 128], F32, st2) for b in range(2)]
            p0 = [R.ps("A_p0%d" % b, [128, 512], F32, st2) for b in range(2)]
            p1 = [R.ps("A_p1", [128, 512], F32, st2)] * 2
            p2 = [R.ps("A_p2", [128, 512], F32, st2)] * 2
            ptrb = [R.ps("A_ptrb%d" % b, [128, 8, 128], BF16, st2) for b in range(2)]
            ptr = [R.ps("A_ptr%d" % b, [128, 8, 128], BF16, st2) for b in range(2)]
            win = w_in.rearrange("(c p) n -> p c n", p=128)
            wdma(g, WA[:, :, 0:512], win[:, :, 0:512], "wa0", ("WA", 0))
            wdma(g, WA[:, :, 512:768], win[:, :, 512:768], "wa1", ("WA", 1))
            wdma(g, WA[:, :, 768:1064], win[:, :, 768:1064], "wa2", ("WA", 2))
            wdma(g, WoA[:], w_out[0:512, :].rearrange("(c p) n -> p c n", p=128), "wa3", "WoA")
            R.op("dve", lambda e: e.memset(vaug[:, :, 1, :], 1.0), writes=["vones"])
            R.op("pool", lambda e: e.memset(kT[:], 0.0), writes=["kzero"])
            for t in range(NT):
                b = t % 2
                tok = slice(t * 128, (t + 1) * 128)
                for (pp, lo, hi, gi) in ((p0, 0, 512, 0), (p1, 512, 768, 1), (p2, 768, 1064, 2)):
                    for c in range(8):
                        R.op("pe", lambda e, pp=pp, lo=lo, hi=hi, c=c, b=b, tok=tok: e.matmul(
                            pp[b][:, 0:hi - lo], lhsT=HT[:, c, tok], rhs=WA[:, c, lo:hi], start=(c == 0), stop=(c == 7)),
                            reads=[("HT", t), ("WA", gi)], writes=[("Ap", gi, b if gi == 0 else 0)])
                R.op("act", lambda e, b=b: e.activation(out=stq[b][:].rearrange("p (g n d) -> p n g d", g=4, n=2),
                                                        in_=p0[b][:].rearrange("p (n g d) -> p n g d", n=2, g=4), func=AF.Copy),
                     reads=[("Ap", 0, b)], writes=[("stq", b)])
                for n_ in range(2):
                    rope_tok(g, p0[b][:, n_ * 256:(n_ + 1) * 256].rearrange("p (h d) -> p h d", h=4),
                             stq[b][:].rearrange("p (g n d) -> p g n d", g=4, n=2)[:, :, n_, :],
                             4, 0, 16, t, tmp, [("Ap", 0, b)], [("stq", b)])
                R.op("act", lambda e, b=b: e.activation(out=stk[b][:], in_=p1[b][:, 0:128], func=AF.Copy), reads=[("Ap", 1, 0)], writes=[("stk", b)])
                rope_tok(g, p1[b][:, 0:128].rearrange("p (h d) -> p h d", h=2), stk[b][:].rearrange("p (h d) -> p h d", h=2),
                         2, 0, 16, t, tmp, [("Ap", 1, 0)], [("stk", b)])
                for n_ in range(2):
                    R.op("act", lambda e, b=b, t=t, n_=n_: e.activation(out=vaug[:, t, 2 * n_, :], in_=p1[b][:, 128 + 64 * n_:192 + 64 * n_], func=AF.Copy),
                         reads=[("Ap", 1, 0)], writes=[("vaug", t)])
                R.op("act", lambda e, b=b: e.activation(out=sti[b][:, 0:288], in_=p2[b][:, 0:288], func=AF.Copy), reads=[("Ap", 2, 0)], writes=[("sti", b)])
                rope_tok(g, p2[b][:, 0:288].rearrange("p (h d) -> p h d", h=9), sti[b][:, 0:288].rearrange("p (h d) -> p h d", h=9),
                         9, 0, 8, t, tmp, [("Ap", 2, 0)], [("sti", b)])
                R.op("act", lambda e, b=b, t=t: e.activation(out=wiS[:, t, :], in_=p2[b][:, 288:296], func=AF.Copy, scale=wi_scale),
                     reads=[("Ap", 2, 0)], writes=[("wiS", t)])
                R.op("dve", lambda e, b=b: e.tensor_copy(out=sti[b][:, 288:384].rearrange("p (r d) -> p r d", r=3),
                                                        in_=sti[b][:, 256:288].unsqueeze(1).to_broadcast([128, 3, 32])),
                     reads=[("sti", b)], writes=[("sti4", b)])
                for gi in range(4):
                    R.op("pe", lambda e, b=b, gi=gi: e.transpose(
                        out=ptr[b][:, gi, :], in_=stq[b][:, gi * 128:(gi + 1) * 128], identity=ident[:]),
                        reads=[("stq", b), "ident"], writes=[("Aptr", b)])
                R.op("pe", lambda e, b=b: e.transpose(out=ptr[b][:, 4, :], in_=stk[b][:], identity=ident[:]), reads=[("stk", b)], writes=[("Aptr", b)])
                for hi_ in range(3):
                    wd_ = 96 if hi_ < 2 else 64
                    R.op("pe", lambda e, b=b, hi_=hi_, wd_=wd_: e.transpose(out=ptrb[b][0:wd_, hi_, :], in_=sti[b][:, hi_ * 96:hi_ * 96 + wd_], identity=ident[:]),
                         reads=[("sti", b)], writes=[("Aptrb", b)])
                R.op("pe", lambda e, b=b: e.transpose(out=ptrb[b][0:96, 3, :], in_=sti[b][:, 288:384], identity=ident[:]), reads=[("sti4", b)], writes=[("Aptrb", b)])
                R.op("act", lambda e, b=b, t=t: e.activation(out=qT[:, t, :, :], in_=ptr[b][:, 0:4, :], func=AF.Copy), reads=[("Aptr", b)], writes=[("AqT", t)])
                R.op("act", lambda e, b=b, tok=tok: e.activation(out=kT[0:64, 0, tok], in_=ptr[b][0:64, 4, :], func=AF.Copy), reads=[("Aptr", b), "kzero"], writes=[("AkT", t)])
                R.op("act", lambda e, b=b, tok=tok: e.activation(out=kT[64:128, 1, tok], in_=ptr[b][64:128, 4, :], func=AF.Copy), reads=[("Aptr", b), "kzero"], writes=[("AkT", t)])
                R.op("act", lambda e, b=b, tok=tok: e.activation(out=qiT[0:96, 0:2, tok], in_=ptrb[b][0:96, 0:2, :], func=AF.Copy), reads=[("Aptrb", b)], writes=[("AqiT", t)])
                R.op("act", lambda e, b=b, tok=tok: e.activation(out=qiT[0:64, 2, tok], in_=ptrb[b][0:64, 2, :], func=AF.Copy), reads=[("Aptrb", b)], writes=[("AqiT", t)])
                R.op("act", lambda e, b=b, tok=tok: e.activation(out=kiT[0:96, tok], in_=ptrb[b][0:96, 3, :], func=AF.Copy), reads=[("Aptrb", b)], writes=[("AkiT", t)])
            R.emit_phase()
        with ExitStack() as st2:
            acc = [R.sb("A_acc%d" % b, [128, S], F32, st2) for b in range(2)]
            junk = R.sb("A_junk", [128, S], mybir.dt.uint8, st2)
            junk2 = junk
            bs = R.sb("A_bs", [128, 2, 8], F32, st2)
            rl = [R.sb("A_rl%d" % b, [128, 512], F32, st2) for b in range(2)]
            KBIS = 28
            P2 = R.sb("A_P2", [128, KBIS], F32, st2)
            wk = R.sb("A_wk", [128, 2, KBIS], F32, st2)
            for k in range(KBIS):
                R.op("pool", lambda e, k=k: e.memset(P2[:, k:k + 1], 2.0 ** -(k + 1)), writes=["P2"])
            maskQ = [R.sb("A_mq%d" % b, [128, S], BF16, st2) for b in range(2)]
            maskT = R.sb("A_mT", [128, NT, 128], BF16, st2)
            E = [R.sb("A_E%d" % b, [128, 512], BF16, st2) for b in range(3)]
            qiz = R.sb("A_qiz", [128, 8, 128], BF16, st2)
            R.op("pool", lambda e: e.memset(qiz[:], 0.0), writes=["qiz0"])
            rden = R.sb("A_rden", [128, 512], F32, st2)
            mixA = [R.sb("A_mix", [128, 4, 128], BF16, st2)] * 2
            pl = [R.ps("A_pl%d" % b, [128, 512], F32, st2) for b in range(2)]
            pmT = R.ps("A_pmT", [128, 8, 128], BF16, st2)
            pS = [R.ps("A_pS%d" % b, [128, 512], F32, st2) for b in range(3)]
            pO = [R.ps("A_pO%d" % b, [128, 512], F32, st2) for b in range(2)]
            pY = pl
            cnt = {"pl": 0, "pS": 0}

            def idx_scores(qb):
                n = 128 * (qb + 1)
                ac = acc[qb % 2]
                ak = ("acc", qb % 2)
                qtok = slice(qb * 128, (qb + 1) * 128)
                for h in range(8):
                    hi_, hp = h // 3, h % 3
                    R.op("pool", lambda e, h=h, hi_=hi_, hp=hp: e.tensor_copy(out=qiz[32 * hp:32 * hp + 32, h, :], in_=qiT[32 * hp:32 * hp + 32, hi_, qtok]),
                         reads=[("AqiT", qb), "qiz0"], writes=[("qiz", h)])
                for kc in range((n + 511) // 512):
                    lo_, hi = kc * 512, min(n, (kc + 1) * 512)
                    w = hi - lo_
                    for h in range(8):
                        hi_, hp = h // 3, h % 3
                        b = cnt["pl"] % 2
                        cnt["pl"] += 1
                        R.op("pe", lambda e, w=w, lo_=lo_, hi=hi, h=h, b=b: e.matmul(
                            pl[b][:, 0:w], lhsT=qiz[0:96, h, :], rhs=kiT[0:96, lo_:hi], start=True, stop=True),
                            reads=[("qiz", h)] + [("AkiT", tt) for tt in range(lo_ // 128, hi // 128)], writes=[("pl", b)])
                        R.op("act", lambda e, b=b, w=w: e.activation(out=rl[b][:, 0:w], in_=pl[b][:, 0:w], func=AF.Relu), reads=[("pl", b)], writes=[("rl", b)])
                        if h == 0:
                            R.op("dve", lambda e, b=b, w=w, lo_=lo_, hi=hi: e.tensor_scalar(out=ac[:, lo_:hi], in0=rl[b][:, 0:w], scalar1=wiS[:, qb, 0:1],
                                                                                         scalar2=None, op0=ALU.mult),
                                 reads=[("rl", b), ("wiS", qb)], writes=[ak])
                        else:
                            R.op("dve", lambda e, b=b, w=w, lo_=lo_, hi=hi, h=h: e.scalar_tensor_tensor(
                                out=ac[:, lo_:hi], in0=rl[b][:, 0:w], scalar=wiS[:, qb, h:h + 1], in1=ac[:, lo_:hi], op0=ALU.mult, op1=ALU.add),
                                reads=[("rl", b), ("wiS", qb), ak], writes=[ak])
                R.op("dve", lambda e: e.tensor_tensor(out=ac[:, n - 128:n], in0=ac[:, n - 128:n], in1=g.NTRI[:], op=ALU.add), reads=[ak], writes=[ak])

            def bisect_pair(qa, hooks=()):
                hooks = list(hooks)
                blocks = [qa, qa + 1]
                if qa >= 2:
                    for x_, qb in enumerate(blocks):
                        n = 128 * (qb + 1)
                        ac, ak = acc[x_], ("acc", x_)
                        R.op("dve", lambda e, ac=ac, n=n, x_=x_: e.tensor_reduce(out=bs[:, x_, 0:1], in_=ac[:, 0:n - 128], axis=AX.X, op=ALU.min),
                             reads=[ak], writes=[("lo", x_)])
                        R.op("dve", lambda e, ac=ac, n=n, x_=x_: e.tensor_reduce(out=bs[:, x_, 1:2], in_=ac[:, 0:n], axis=AX.X, op=ALU.max),
                             reads=[ak], writes=[("w0", x_)])
                    for x_ in range(2):
                        R.op("dve", lambda e, x_=x_: e.tensor_tensor(out=bs[:, x_, 1:2], in0=bs[:, x_, 1:2], in1=bs[:, x_, 0:1], op=ALU.subtract),
                             reads=[("w0", x_), ("lo", x_)], writes=[("w0", x_)])
                    for x_ in range(2):
                        R.op("dve", lambda e, x_=x_: e.tensor_scalar(out=wk[:, x_, :], in0=P2[:], scalar1=bs[:, x_, 1:2], scalar2=None, op0=ALU.mult),
                             reads=[("w0", x_), "P2"], writes=[("wk", x_)])
                    for k in range(KBIS):
                        if hooks and k == ((KBIS // 3) if len(hooks) == 2 else (KBIS - 1)):
                            hooks.pop(0)()
                        R.op("dve", lambda e, k=k: e.tensor_tensor(out=bs[:, 0, 2:3], in0=wk[:, 0, k:k + 1], in1=bs[:, 0, 0:1], op=ALU.add),
                             reads=[("wk", 0), ("lo", 0)], writes=[("mid", 0)])
                        R.op("dve", lambda e, k=k: e.scalar_tensor_tensor(out=bs[:, 1, 2:3], in0=wk[:, 1, k:k + 1], scalar=-1.0, in1=bs[:, 1, 0:1],
                                                                          op0=ALU.mult, op1=ALU.subtract), reads=[("wk", 1), ("lo", 1)], writes=[("mid", 1)])
                        n0, n1 = 128 * (blocks[0] + 1), 128 * (blocks[1] + 1)
                        R.op("act", lambda e, n1=n1: e.activation(out=junk[:, 0:n1], in_=acc[1][:, 0:n1], func=AF.Sign, bias=bs[:, 1, 2:3], scale=1.0,
                                                                  accum_out=bs[:, 1, 3:4]), reads=[("acc", 1), ("mid", 1)], writes=[("cnt", 1), "junkA"])
                        R.op("dve", lambda e, n0=n0: e.tensor_scalar(out=maskQ[0][:, 0:n0], in0=acc[0][:, 0:n0], scalar1=bs[:, 0, 2:3], scalar2=None,
                                                                     op0=ALU.is_ge, op1=ALU.add, accum_out=bs[:, 0, 3:4]),
                             reads=[("acc", 0), ("mid", 0)], writes=[("cnt", 0), ("mq", 0)])
                        R.op("dve", lambda e, k=k: e.scalar_tensor_tensor(out=bs[:, 0, 4:5], in0=bs[:, 0, 3:4], scalar=255.5, in1=wk[:, 0, k:k + 1],
                                                                          op0=ALU.is_ge, op1=ALU.mult), reads=[("cnt", 0), ("wk", 0)], writes=[("sel", 0)])
                        R.op("dve", lambda e, k=k, n1=n1: e.scalar_tensor_tensor(out=bs[:, 1, 4:5], in0=bs[:, 1, 3:4], scalar=511.0 - n1, in1=wk[:, 1, k:k + 1],
                                                                                 op0=ALU.is_ge, op1=ALU.mult), reads=[("cnt", 1), ("wk", 1)], writes=[("sel", 1)])
                        for x_ in range(2):
                            R.op("dve", lambda e, x_=x_: e.tensor_tensor(out=bs[:, x_, 0:1], in0=bs[:, x_, 0:1], in1=bs[:, x_, 4:5], op=ALU.add),
                                 reads=[("lo", x_), ("sel", x_)], writes=[("lo", x_)])
                    for x_, qb in enumerate(blocks):
                        n = 128 * (qb + 1)
                        R.op("dve", lambda e, x_=x_, n=n: e.tensor_scalar(out=maskQ[x_][:, 0:n], in0=acc[x_][:, 0:n], scalar1=bs[:, x_, 0:1], scalar2=None, op0=ALU.is_ge),
                             reads=[("acc", x_), ("lo", x_)], writes=[("mq", x_)])
                else:
                    for x_, qb in enumerate(blocks):
                        n = 128 * (qb + 1)
                        R.op("dve", lambda e, x_=x_, n=n: e.tensor_scalar(out=maskQ[x_][:, 0:n], in0=acc[x_][:, 0:n], scalar1=NEG, scalar2=None, op0=ALU.is_gt),
                             reads=[("acc", x_)], writes=[("mq", x_)])
                while hooks:
                    hooks.pop(0)()

            def attend_main(qb):
                mq = maskQ[qb % 2]
                for j0 in range(0, qb + 1, 8):
                    j1 = min(qb + 1, j0 + 8)
                    for j in range(j0, j1):
                        R.op("pe", lambda e, j=j, j0=j0: e.transpose(out=pmT[:, j - j0, :], in_=mq[:, j * 128:(j + 1) * 128], identity=ident[:]),
                             reads=[("mq", qb % 2)], writes=["pmT"])
                    R.op("act", lambda e, j0=j0, j1=j1: e.activation(out=maskT[:, j0:j1, :], in_=pmT[:, 0:j1 - j0, :], func=AF.Copy),
                         reads=["pmT"], writes=["maskT"])
                steps = [(n_, j) for n_ in range(2) for j in range(qb + 1)]
                NB = len(pS)
                LOOK = NB - 1

                def qk(s_):
                    n_, j = steps[s_]
                    b = cnt["pS"] % NB
                    cnt["pS"] += 1
                    R.op("pe", lambda e: e.matmul(pS[b][:], lhsT=kT[:, n_, j * 128:(j + 1) * 128], rhs=qT[:, qb, :, :],
                                                  start=True, stop=True), reads=[("AkT", j), ("AqT", qb)], writes=[("pS", b)])
                    R.op("act", lambda e: e.activation(out=E[b][:], in_=pS[b][:], func=AF.Exp, scale=0.125), reads=[("pS", b)], writes=[("E", b)])
                    R.op("pool", lambda e: e.tensor_tensor(
                        out=E[b][:].rearrange("p (h q) -> p h q", h=4), in0=E[b][:].rearrange("p (h q) -> p h q", h=4),
                        in1=maskT[:, j, :].unsqueeze(1).to_broadcast([128, 4, 128]), op=ALU.mult),
                        reads=[("E", b), "maskT"], writes=[("E", b)])
                    return b

                pend = {}
                for s_ in range(len(steps) + LOOK):
                    if s_ < len(steps):
                        pend[s_] = qk(s_)
                    if s_ - LOOK >= 0:
                        n_, j = steps[s_ - LOOK]
                        b = pend.pop(s_ - LOOK)
                        R.op("pe", lambda e, b=b, j=j, n_=n_: e.matmul(pO[n_][:], lhsT=vaug[:, j, n_:n_ + 2, :], rhs=E[b][:], start=(j == 0), stop=(j == qb)),
                             reads=[("E", b), ("vaug", j), "vones"], writes=[("pO", n_)])

            def attend_dve(qb):
                mx = mixA[qb % 2]
                for n_ in range(2):
                    bo = n_
                    orow = 64 * n_
                    drow = 64 - orow
                    R.op("dve", lambda e, bo=bo, drow=drow: e.reciprocal(out=rden[drow:drow + 64, :], in_=pO[bo][drow:drow + 64, :]), reads=[("pO", bo)], writes=["rden"])
                    for base in range(2):
                        R.op("dve", lambda e, bo=bo, base=base, n_=n_, orow=orow, drow=drow: e.tensor_tensor(
                            out=mx[base * 64:(base + 1) * 64, 2 * n_:2 * n_ + 2, :],
                            in0=pO[bo][orow:orow + 64, :].rearrange("p (a b q) -> p a b q", a=2, b=2)[:, :, base, :],
                            in1=rden[drow:drow + 64, :].rearrange("p (a b q) -> p a b q", a=2, b=2)[:, :, base, :], op=ALU.mult),
                            reads=[("pO", bo), "rden"], writes=[("mixA", qb % 2)])
                outproj_add(g, lambda k: mx[:, k, :], 4, WoA, "WoA", qb, pY, [("mixA", qb % 2)], pykey="pl")

            idx_scores(0)
            idx_scores(1)
            bisect_pair(0)
            for p in range(NT // 2):
                qa = 2 * p
                if p + 1 < NT // 2:
                    idx_scores(qa + 2)
                    idx_scores(qa + 3)
                attend_main(qa)
                h1 = lambda qa=qa: (attend_dve(qa), attend_main(qa + 1))
                h2 = lambda qa=qa: attend_dve(qa + 1)
                if p + 1 < NT // 2:
                    bisect_pair(qa + 2, hooks=(h1, h2))
                else:
                    h1()
                    h2()
            R.emit_phase()


def phase_B(g, i):
    R = g.R
    HT, X, ident = g.HT, g.X, g.ident
    w_in = g.W['ev_w_in'][i].rearrange("(c p) n -> p c n", p=128)
    w_out = g.W['ev_w_out'][i]
    with ExitStack() as st:
        WB = [R.sb("B_W%d" % b, [128, 8, 384], BF16, st) for b in range(2)]
        WoB = [R.sb("B_Wo%d" % b, [128, 1, D], BF16, st) for b in range(2)]
        qkT = [R.sb("B_qkT%d" % b, [128, 3, S], BF16, st) for b in range(2)]
        vaug = [R.sb("B_vaug%d" % b, [128, NT, 2, 128], BF16, st) for b in range(2)]
        mixp = [R.sb("B_mix%d" % b, [128, S], BF16, st) for b in range(2)]
        stg = [R.sb("B_st%d" % b, [128, 256], BF16, st) for b in range(2)]
        tmp = [R.sb("B_rt%d" % b, [128, 128], F32, st) for b in range(2)]
        E = [R.sb("B_E%d" % b, [128, 512], BF16, st) for b in range(3)]
        P = [R.sb("B_P%d" % b, [128, 512], BF16, st) for b in range(3)]
        rden = R.sb("B_rden", [128, 512], F32, st)
        pp = [R.ps("B_pp", [128, 512], F32, st)] * 2
        ptr = R.ps("B_ptr", [128, 8, 128], BF16, st)
        pS = [R.ps("B_pS%d" % b, [128, 512], F32, st) for b in range(3)]
        pO = [R.ps("B_pO%d" % b, [128, 512], F32, st) for b in range(2)]
        pY = [R.ps("B_pY", [128, 512], F32, st)]
        for b in range(2):
            R.op("dve", lambda e, b=b: e.memset(vaug[b][:, :, :, 64:128], 1.0), writes=[("vones", b)])
            R.op("pool", lambda e, b=b: e.memset(qkT[b][:, 0:2, :], 0.0), writes=[("qzero", b)])

        ppS = [R.sb("B_ppS%d" % b, [128, 384], F32, st) for b in range(2)]

        def issue_w(p):
            pb = p % 2
            for k3, off in enumerate((1064, 1576, 2088)):
                wdma(g, WB[pb][:, :, k3 * 128:(k3 + 1) * 128], w_in[:, :, off + p * 128:off + (p + 1) * 128], "wb%d_%d" % (pb, k3), ("WB", pb, k3))

        def issue_wo(p):
            pb = p % 2
            wdma(g, WoB[pb][:, 0, :], w_out[512 + p * 128:512 + (p + 1) * 128, :], "wob%d" % pb, ("WoB", pb))

        def proj(p):
            pb = p % 2
            for t in range(NT):
                b = t % 2
                tok = slice(t * 128, (t + 1) * 128)
                for c in range(8):
                    R.op("pe", lambda e, c=c, b=b, tok=tok: e.matmul(pp[b][:, 0:384], lhsT=HT[:, c, tok], rhs=WB[pb][:, c, :], start=(c == 0), stop=(c == 7)),
                         reads=[("HT", t)] + [("WB", pb, k3) for k3 in range(3)], writes=[("pp", 0)])
                    yield
                R.op("act", lambda e, b=b: e.activation(out=ppS[b][:], in_=pp[b][:, 0:384], func=AF.Copy), reads=[("pp", 0)], writes=[("ppS", b)])
                R.op("pool", lambda e, b=b: e.tensor_copy(out=stg[b][:], in_=ppS[b][:, 0:256]), reads=[("ppS", b)], writes=[("stg", b)])
                rope_tok(g, ppS[b][:, 0:256].rearrange("p (h d) -> p h d", h=4), stg[b][:].rearrange("p (h d) -> p h d", h=4),
                         4, 0, 16, t, tmp, [("ppS", b)], [("stg", b)])
                R.op("pool", lambda e, b=b, t=t: e.tensor_copy(out=vaug[pb][:, t, :, 0:64], in_=ppS[b][:, 256:384].rearrange("p (h d) -> p h d", h=2)),
                     reads=[("ppS", b)], writes=[("Bv", pb)])
                yield
                if t > 0:
                    xpose(pb, t - 1)
                    yield
            xpose(pb, NT - 1)
            yield

        def xpose(pb, t):
            b = t % 2
            tok = slice(t * 128, (t + 1) * 128)
            for x_ in range(2):
                R.op("pe", lambda e, b=b, x_=x_: e.transpose(out=ptr[:, x_, :], in_=stg[b][:, x_ * 128:(x_ + 1) * 128], identity=ident[:]),
                     reads=[("stg", b)], writes=["Bptr"])
            R.op("act", lambda e, tok=tok: e.activation(out=qkT[pb][0:64, 0, tok], in_=ptr[0:64, 0, :], func=AF.Copy), reads=["Bptr", ("qzero", pb)], writes=[("BqkT", pb)])
            R.op("act", lambda e, tok=tok: e.activation(out=qkT[pb][64:128, 1, tok], in_=ptr[64:128, 0, :], func=AF.Copy), reads=["Bptr", ("qzero", pb)], writes=[("BqkT", pb)])
            R.op("act", lambda e, tok=tok: e.activation(out=qkT[pb][:, 2, tok], in_=ptr[:, 1, :], func=AF.Copy), reads=["Bptr"], writes=[("BqkT", pb)])

        def outproj_gen(p, tiles=range(NT)):
            pb = p % 2
            for t in tiles:
                c = t // 4
                outproj_add(g, lambda k, t=t: mixp[pb][:, t * 128:(t + 1) * 128], 1, WoB[pb], ("WoB", pb), t, pY,
                            [("Bmix%d" % pb, c, 0), ("Bmix%d" % pb, c, 1)])
                yield

        def attn(p, filler=None):
            pb = p % 2
            own = (lambda c: outproj_gen(p, range(4 * c, 4 * c + 4))) if p == 3 else None
            attn_pair(g, "B", lambda hh, q0, N: qkT[pb][:, hh, q0:q0 + N],
                      lambda hh, j: qkT[pb][:, 2, j * 128:(j + 1) * 128],
                      lambda hh, j: vaug[pb][:, j, hh, :],
                      [("BqkT", pb), ("Bv", pb), ("vones", pb)], 0.125, "B", mixp[pb], "Bmix%d" % pb, pS, pO, E, P, rden, None,
                      mask_eng=("dve",), filler=filler, own_outproj=own)

        issue_w(0)
        issue_w(1)
        issue_wo(0)
        for _ in proj(0):
            pass
        og = iter(())
        for p in range(4):
            gen = proj(p + 1) if p + 1 < 4 else iter(())
            if p + 2 < 4:
                issue_w(p + 2)
            st_ = {"i": 0}

            def filler(gen=gen, og=og, st_=st_):
                st_["i"] += 1
                next(gen, None)
                if st_["i"] % 8 == 0:
                    next(og, None)

            attn(p, filler=filler)
            for _ in gen:
                pass
            for _ in og:
                pass
            if p + 1 < 4:
                issue_wo(p + 1)
            og = outproj_gen(p) if p < 3 else iter(())
        R.emit_phase()


def ffn_core(g, st, wg, wu, wd, gate_fn, bufs):
    R = g.R
    HT, X = g.HT, g.X
    Wg, Wu, Wd, AT, sg, pG, pU, pY = bufs
    wgv = wg.rearrange("(c p) n -> p c n", p=128)
    wuv = wu.rearrange("(c p) n -> p c n", p=128)
    wdv = wd.rearrange("(c p) n -> p c n", p=128)
    cnt = g.ffn_cnt
    for f in range(NSLAB):
        sb_ = cnt["slab"] % 2
        cnt["slab"] += 1
        wdma(g, Wg[sb_][:], wgv[:, :, f * 512:(f + 1) * 512], "wg%d" % sb_, ("Wg", sb_))
        wdma(g, Wu[sb_][:], wuv[:, :, f * 512:(f + 1) * 512], "wu%d" % sb_, ("Wu", sb_))
        wdma(g, Wd[sb_][:], wdv[:, f * 4:(f + 1) * 4, :], "wd%d" % sb_, ("Wd", sb_))
        for tg in range(4):
            toks = slice(tg * 512, (tg + 1) * 512)
            hkeys = [("HT", t) for t in range(4 * tg, 4 * tg + 4)]
            for ch in range(4):
                b = cnt["gu"] % 2
                cnt["gu"] += 1
                for (pt, Wt, wk, nm) in ((pG, Wg, "Wg", "pG"), (pU, Wu, "Wu", "pU")):
                    for c in range(8):
                        R.op("pe", lambda e, pt=pt, Wt=Wt, c=c, b=b, ch=ch, toks=toks, sb_=sb_: e.matmul(
                            pt[b][:], lhsT=Wt[sb_][:, c, ch * 128:(ch + 1) * 128], rhs=HT[:, c, toks], start=(c == 0), stop=(c == 7)),
                            reads=hkeys + [(wk, sb_)], writes=[(nm, b)])
                R.op("act", lambda e, b=b: e.activation(out=sg[b][:], in_=pG[b][:], func=AF.Silu), reads=[("pG", b)], writes=[("sg", b)])
                R.op("dve", lambda e, b=b, ch=ch, toks=toks: e.tensor_tensor(out=AT[:, ch, toks], in0=pU[b][:], in1=sg[b][:], op=ALU.mult),
                     reads=[("pU", b), ("sg", b)], writes=[("AT", tg)])
        for t in range(NT):
            tok = slice(t * 128, (t + 1) * 128)
            for half in range(2):
                b = cnt["y"] % 2
                cnt["y"] += 1
                hs = slice(half * 512, (half + 1) * 512)
                for ch in range(4):
                    R.op("pe", lambda e, b=b, ch=ch, tok=tok, hs=hs, sb_=sb_: e.matmul(pY[b][:], lhsT=AT[:, ch, tok], rhs=Wd[sb_][:, ch, hs],
                                                                                start=(ch == 0), stop=(ch == 3)),
                         reads=[("AT", t // 4), ("Wd", sb_)], writes=[("pY", b)])
                if gate_fn is None:
                    R.op("dve", lambda e, b=b, t=t, hs=hs: e.tensor_tensor(out=X[:, t, hs], in0=pY[b][:], in1=X[:, t, hs], op=ALU.add),
                         reads=[("pY", b), ("X", t, half)], writes=[("X", t, half)])
                else:
                    R.op("dve", lambda e, b=b, t=t, hs=hs: e.scalar_tensor_tensor(out=X[:, t, hs], in0=pY[b][:], scalar=gate_fn(t), in1=X[:, t, hs],
                                                                                 op0=ALU.mult, op1=ALU.add),
                         reads=[("pY", b), ("X", t, half), "gates"], writes=[("X", t, half)])


def ffn_bufs(g, st):
    R = g.R
    Wg = [R.sb("F_Wg%d" % b, [128, 8, 512], BF16, st) for b in range(2)]
    Wu = [R.sb("F_Wu%d" % b, [128, 8, 512], BF16, st) for b in range(2)]
    Wd = [R.sb("F_Wd%d" % b, [128, 4, D], BF16, st) for b in range(2)]
    AT = R.sb("F_AT", [128, 4, S], BF16, st)
    sg = [R.sb("F_sg%d" % b, [128, 512], BF16, st) for b in range(2)]
    pG = [R.ps("F_pG%d" % b, [128, 512], F32, st) for b in range(2)]
    pU = [R.ps("F_pU%d" % b, [128, 512], F32, st) for b in range(2)]
    pY = [R.ps("F_pY%d" % b, [128, 512], F32, st) for b in range(2)]
    g.ffn_cnt = {"slab": 0, "gu": 0, "y": 0}
    return (Wg, Wu, Wd, AT, sg, pG, pU, pY)


def phase_ffn(g, wg, wu, wd, gate_fn, norm_gain=None, tail=None):
    with ExitStack() as st:
        bufs = ffn_bufs(g, st)
        if norm_gain is not None:
            phase_norm(g, norm_gain, st=st)
        ffn_core(g, st, wg, wu, wd, gate_fn, bufs)
        if tail is not None:
            tail(st)
        g.R.emit_phase()


def phase_moe(g, i, norm_gain=None, tail=None, ntail=None):
    R = g.R
    HT = g.HT
    with ExitStack() as st:
        if norm_gain is not None:
            phase_norm(g, norm_gain, st=st)
        WR = R.sb("M_WR", [128, 8, 8], BF16, st)
        lg = R.sb("M_lg", [128, NT, 8], F32, st)
        gates = R.sb("M_gates", [128, NT, 8], F32, st)
        m8 = R.sb("M_m8", [128, NT, 8], F32, st)
        s12 = R.sb("M_s12", [128, NT], F32, st)
        z = R.sb("M_z", [128, NT, 8], F32, st)
        msk = R.sb("M_msk", [128, NT, 8], F32, st)
        bufs = ffn_bufs(g, st)
        pR = bufs[5][0]
        wdma(g, WR[:], g.W['moe_router'][i].rearrange("(c p) e -> p c e", p=128), "wr", "WR")
        for t in range(NT):
            tok = slice(t * 128, (t + 1) * 128)
            for c in range(8):
                R.op("pe", lambda e, c=c, tok=tok, t=t: e.matmul(pR[:, t * 8:(t + 1) * 8], lhsT=HT[:, c, tok], rhs=WR[:, c, :], start=(c == 0), stop=(c == 7)),
                     reads=[("HT", t), "WR"], writes=[("pG", 0)])
        R.op("act", lambda e: e.activation(out=lg[:].rearrange("p t e -> p (t e)"), in_=pR[:, 0:NT * 8], func=AF.Copy), reads=[("pG", 0)], writes=["lg"])
        for t in range(NT):
            R.op("dve", lambda e, t=t: e.max(out=m8[:, t, :], in_=lg[:, t, :]), reads=["lg"], writes=["m8"])
        R.op("dve", lambda e: e.tensor_tensor(out=s12[:], in0=m8[:, :, 0], in1=m8[:, :, 1], op=ALU.add), reads=["m8"], writes=["s12"])
        R.op("dve", lambda e: e.scalar_tensor_tensor(out=z[:], in0=lg[:], scalar=2.0, in1=s12[:].unsqueeze(2).to_broadcast([128, NT, 8]),
                                                     op0=ALU.mult, op1=ALU.subtract), reads=["lg", "s12"], writes=["z"])
        R.op("act", lambda e: e.activation(out=z[:], in_=z[:], func=AF.Sigmoid), reads=["z"], writes=["z"])
        R.op("dve", lambda e: e.tensor_tensor(out=msk[:], in0=lg[:], in1=m8[:, :, 1:2].to_broadcast([128, NT, 8]), op=ALU.is_ge),
             reads=["lg", "m8"], writes=["msk"])
        R.op("dve", lambda e: e.tensor_tensor(out=gates[:], in0=z[:], in1=msk[:], op=ALU.mult), reads=["z", "msk"], writes=["gates"])
        for ex in range(g.n_experts):
            ffn_core(g, st, g.W['moe_w_gate'][i, ex], g.W['moe_w_up'][i, ex], g.W['moe_w_down'][i, ex],
                     lambda t, ex=ex: gates[:, t, ex:ex + 1], bufs)
        if tail is not None:
            tail(st)
        if ntail is not None:
            ntail(st)
        R.emit_phase(last=(tail is not None))


def phase_final(g, do_final, st=None):
    R = g.R
    X, gbc, ss, rstd = g.X, g.gbc, g.ss, g.rstd
    outv = g.out.rearrange("(t p) d -> p t d", p=128)
    ext = st
    with ExitStack() as st_:
        st = st_ if ext is None else ext
        if do_final:
            junk = R.sb("fjunk", [128, D], BF16, st)
            ob = [R.sb("fob%d" % b, [128, D], F32, st) for b in range(2)]
            R.dma("sp", lambda e: e.dma_start(out=gbc[:], in_=g.W['final_norm'][0:1, :].to_broadcast([128, D])), "gbc", writes=["gbc"])
            for t in range(NT):
                R.op("act", lambda e, t=t: e.activation(out=junk[:], in_=X[:, t, :], func=AF.Square, accum_out=ss[:, t:t + 1]),
                     reads=[("X", t, 0), ("X", t, 1)], writes=["junk", ("ss", t)])
            allss = [("ss", t) for t in range(NT)]
            R.op("dve", lambda e: e.tensor_scalar(out=rstd[:], in0=ss[:], scalar1=1.0 / D, scalar2=EPS, op0=ALU.mult, op1=ALU.add),
                 reads=allss, writes=["rstd"])
            R.op("act", lambda e: e.activation(out=rstd[:], in_=rstd[:], func=AF.Sqrt), reads=["rstd"], writes=["rstd"])
            R.op("dve", lambda e: e.reciprocal(out=rstd[:], in_=rstd[:]), reads=["rstd"], writes=["rstd"])
            for t in range(NT):
                b = t % 2
                R.op("dve", lambda e, t=t, b=b: e.scalar_tensor_tensor(out=ob[b][:], in0=X[:, t, :], scalar=rstd[:, t:t + 1], in1=gbc[:],
                                                                       op0=ALU.mult, op1=ALU.mult),
                     reads=[("X", t, 0), ("X", t, 1), "rstd", "gbc"], writes=[("ob", b)])
                R.dma("sp", lambda e, t=t, b=b: e.dma_start(out=outv[:, t, :], in_=ob[b][:]), "out%d" % b, reads=[("ob", b)], final=True)
        else:
            for t in range(NT):
                R.dma("sp", lambda e, t=t: e.dma_start(out=outv[:, t, :], in_=X[:, t, :]), "out%d" % (t % 2),
                      reads=[("X", t, 0), ("X", t, 1)], final=True)
        if ext is None:
            R.emit_phase(last=True)


def phase_C(g, i, norm_gain=None):
    R = g.R
    HT, X, ident = g.HT, g.X, g.ident
    w_in = g.W['od_w_in'][i].rearrange("(c p) n -> p c n", p=128)
    w_out = g.W['od_w_out'][i]
    w_uq = g.W['mla_w_uq'][i].rearrange("(c p) n -> p c n", p=128)
    w_ukv = g.W['mla_w_ukv'][i]
    scale = 96.0 ** -0.5
    with ExitStack() as st:
        cT = R.sb("C_cT", [128, 3, S], BF16, st)
        krS = R.sb("C_krS", [128, NT, 32], BF16, st)
        with ExitStack() as st2:
            WC = R.sb("C_W", [128, 8, 416], BF16, st2)
            gq = R.sb("C_gq", [128, 256], F32, st2)
            gkv = R.sb("C_gkv", [128, 128], F32, st2)
            junk = R.sb("C_junk", [128, 256], BF16, st2)
            ssq = [R.sb("C_ssq%d" % b, [128, 2], F32, st2) for b in range(2)]
            stc = [R.sb("C_stc%d" % b, [128, 384], BF16, st2) for b in range(2)]
            tmp = [R.sb("C_rt%d" % b, [128, 128], F32, st2) for b in range(2)]
            pc = [R.ps("C_pc%d" % b, [128, 512], F32, st2) for b in range(2)]
            ptr = [R.ps("C_ptr%d" % b, [128, 8, 128], BF16, st2) for b in range(2)]
            wdma(g, WC[:], w_in[:, :, 0:416], "wc", "WC")
            if norm_gain is not None:
                phase_norm(g, norm_gain, st=st2)
            R.dma("sp", lambda e: e.dma_start(out=gq[:], in_=g.W['mla_q_norm'][i:i + 1, :].to_broadcast([128, 256])), "gq", writes=["gq"])
            R.dma("sp", lambda e: e.dma_start(out=gkv[:], in_=g.W['mla_kv_norm'][i:i + 1, :].to_broadcast([128, 128])), "gkv", writes=["gkv"])
            for t in range(NT):
                b = t % 2
                tok = slice(t * 128, (t + 1) * 128)
                for c in range(8):
                    R.op("pe", lambda e, c=c, b=b, tok=tok: e.matmul(pc[b][:, 0:416], lhsT=HT[:, c, tok], rhs=WC[:, c, :], start=(c == 0), stop=(c == 7)),
                         reads=[("HT", t), "WC"], writes=[("pc", b)])
                R.op("act", lambda e, b=b: e.activation(out=junk[:, 0:256], in_=pc[b][:, 0:256], func=AF.Square, accum_out=ssq[b][:, 0:1]),
                     reads=[("pc", b)], writes=["junk", ("ssq", b, 0)])
                R.op("act", lambda e, b=b: e.activation(out=junk[:, 0:128], in_=pc[b][:, 256:384], func=AF.Square, accum_out=ssq[b][:, 1:2]),
                     reads=[("pc", b)], writes=["junk", ("ssq", b, 1)])
                R.op("dve", lambda e, b=b: e.tensor_scalar(out=ssq[b][:, 0:1], in0=ssq[b][:, 0:1], scalar1=1.0 / 256, scalar2=EPS, op0=ALU.mult, op1=ALU.add),
                     reads=[("ssq", b, 0)], writes=[("ssq", b, 0)])
                R.op("dve", lambda e, b=b: e.tensor_scalar(out=ssq[b][:, 1:2], in0=ssq[b][:, 1:2], scalar1=1.0 / 128, scalar2=EPS, op0=ALU.mult, op1=ALU.add),
                     reads=[("ssq", b, 1)], writes=[("ssq", b, 1)])
                R.op("act", lambda e, b=b: e.activation(out=ssq[b][:], in_=ssq[b][:], func=AF.Sqrt), reads=[("ssq", b, 0), ("ssq", b, 1)], writes=[("ssq", b, 2)])
                R.op("dve", lambda e, b=b: e.reciprocal(out=ssq[b][:], in_=ssq[b][:]), reads=[("ssq", b, 2)], writes=[("ssq", b, 3)])
                R.op("dve", lambda e, b=b: e.scalar_tensor_tensor(out=stc[b][:, 0:256], in0=pc[b][:, 0:256], scalar=ssq[b][:, 0:1], in1=gq[:],
                                                                  op0=ALU.mult, op1=ALU.mult), reads=[("pc", b), ("ssq", b, 3), "gq"], writes=[("stc", b)])
                R.op("dve", lambda e, b=b: e.scalar_tensor_tensor(out=stc[b][:, 256:384], in0=pc[b][:, 256:384], scalar=ssq[b][:, 1:2], in1=gkv[:],
                                                                  op0=ALU.mult, op1=ALU.mult), reads=[("pc", b), ("ssq", b, 3), "gkv"], writes=[("stc", b)])
                rope_tok(g, pc[b][:, 384:416].rearrange("p (h d) -> p h d", h=1), krS[:, t, :].rearrange("p (h d) -> p h d", h=1),
                         1, 0, 32, t, tmp, [("pc", b)], [("krS", t)])
                for x_ in range(3):
                    R.op("pe", lambda e, b=b, x_=x_: e.transpose(out=ptr[b][:, x_, :], in_=stc[b][:, x_ * 128:(x_ + 1) * 128], identity=ident[:]),
                         reads=[("stc", b)], writes=[("Cptr", b)])
                R.op("act", lambda e, b=b, tok=tok: e.activation(out=cT[:, :, tok], in_=ptr[b][:, 0:3, :], func=AF.Copy), reads=[("Cptr", b)], writes=[("cT", t)])
            R.emit_phase()
        with ExitStack() as st2:
            Wuq = [R.sb("C_Wuq%d" % b, [128, 2, 192], BF16, st2) for b in range(2)]
            Wukv = [R.sb("C_Wukv%d" % b, [128, 256], BF16, st2) for b in range(2)]
            WoC = [R.sb("C_Wo%d" % b, [128, 1, D], BF16, st2) for b in range(2)]
            qkT = [R.sb("C_qkT%d" % b, [128, 4, S], BF16, st2) for b in range(2)]
            vaug = [R.sb("C_vaug%d" % b, [128, NT, 2, 128], BF16, st2) for b in range(2)]
            mixp = [R.sb("C_mix%d" % b, [128, S], BF16, st2) for b in range(2)]
            ppS = [R.sb("C_ppS%d" % b, [128, 448], F32, st2) for b in range(2)]
            stq = [R.sb("C_stq%d" % b, [128, 192], BF16, st2) for b in range(2)]
            stk = [R.sb("C_stk%d" % b, [128, 2, 96], BF16, st2) for b in range(2)]
            tmp = [R.sb("C_rt2%d" % b, [128, 128], F32, st2) for b in range(2)]
            E = [R.sb("C_E%d" % b, [128, 512], BF16, st2) for b in range(3)]
            P = [R.sb("C_P%d" % b, [128, 512], BF16, st2) for b in range(3)]
            rden = R.sb("C_rden", [128, 512], F32, st2)
            pqkv = R.ps("C_pqkv", [128, 512], F32, st2)
            ptr = R.ps("C_ptr2", [128, 8, 128], BF16, st2)
            pS = [R.ps("C_pS%d" % b, [128, 512], F32, st2) for b in range(3)]
            pO = [R.ps("C_pO%d" % b, [128, 512], F32, st2) for b in range(2)]
            pY = [R.ps("C_pY", [128, 512], F32, st2)]
            for b in range(2):
                R.op("dve", lambda e, b=b: e.memset(vaug[b][:, :, :, 64:128], 1.0), writes=[("vones", b)])
                R.op("pool", lambda e, b=b: e.memset(qkT[b][:], 0.0), writes=[("CqkT", b)])

            def issue_w(p):
                pb = p % 2
                wdma(g, Wuq[pb][:], w_uq[:, :, p * 192:(p + 1) * 192], "wuq%d" % pb, ("Wuq", pb))
                wdma(g, Wukv[pb][:], w_ukv[:, p * 256:(p + 1) * 256], "wukv%d" % pb, ("Wukv", pb))

            def issue_wo(p):
                pb = p % 2
                wdma(g, WoC[pb][:, 0, :], w_out[p * 128:(p + 1) * 128, :], "woc%d" % pb, ("WoC", pb))

            def proj(p):
                pb = p % 2
                for t in range(NT):
                    b = t % 2
                    tok = slice(t * 128, (t + 1) * 128)
                    for c in range(2):
                        R.op("pe", lambda e, c=c, tok=tok: e.matmul(pqkv[:, 0:192], lhsT=cT[:, c, tok], rhs=Wuq[pb][:, c, :], start=(c == 0), stop=(c == 1)),
                             reads=[("cT", t), ("Wuq", pb)], writes=["pq"])
                        yield
                    R.op("pe", lambda e, tok=tok: e.matmul(pqkv[:, 192:448], lhsT=cT[:, 2, tok], rhs=Wukv[pb][:], start=True, stop=True),
                         reads=[("cT", t), ("Wukv", pb)], writes=["pq"])
                    yield
                    R.op("act", lambda e, b=b: e.activation(out=ppS[b][:], in_=pqkv[:, 0:448], func=AF.Copy), reads=["pq"], writes=[("ppS", b)])
                    R.op("pool", lambda e, b=b: e.tensor_copy(out=stq[b][:], in_=ppS[b][:, 0:192]), reads=[("ppS", b)], writes=[("Cstq", b)])
                    rope_tok(g, ppS[b][:, 0:192].rearrange("p (h d) -> p h d", h=2), stq[b][:].rearrange("p (h d) -> p h d", h=2),
                             2, 64, 32, t, tmp, [("ppS", b)], [("Cstq", b)])
                    R.op("pool", lambda e, b=b: e.tensor_copy(out=stk[b][:, :, 0:64], in_=ppS[b][:, 192:448].rearrange("p (h d) -> p h d", h=2)[:, :, 0:64]),
                         reads=[("ppS", b)], writes=[("Cstk", b)])
                    R.op("pool", lambda e, b=b, t=t: e.tensor_copy(out=stk[b][:, :, 64:96], in_=krS[:, t, :].unsqueeze(1).to_broadcast([128, 2, 32])),
                         reads=[("krS", t)], writes=[("Cstk", b)])
                    R.op("pool", lambda e, b=b, t=t: e.tensor_copy(out=vaug[pb][:, t, :, 0:64], in_=ppS[b][:, 192:448].rearrange("p (h d) -> p h d", h=2)[:, :, 64:128]),
                         reads=[("ppS", b)], writes=[("Cv", pb)])
                    yield
                    if t > 0:
                        xpose(pb, t - 1)
                        yield
                xpose(pb, NT - 1)
                yield

            def xpose(pb, t):
                b = t % 2
                tok = slice(t * 128, (t + 1) * 128)
                for hh in range(2):
                    R.op("pe", lambda e, b=b, hh=hh: e.transpose(out=ptr[0:96, hh, :], in_=stq[b][:, hh * 96:(hh + 1) * 96], identity=ident[:]),
                         reads=[("Cstq", b)], writes=["Cptr2"])
                    R.op("pe", lambda e, b=b, hh=hh: e.transpose(out=ptr[0:96, 2 + hh, :], in_=stk[b][:, hh, :], identity=ident[:]),
                         reads=[("Cstk", b)], writes=["Cptr2"])
                R.op("act", lambda e, tok=tok: e.activation(out=qkT[pb][0:96, :, tok], in_=ptr[0:96, 0:4, :], func=AF.Copy), reads=["Cptr2"], writes=[("CqkT", pb)])

            def outproj_gen(p, tiles=range(NT)):
                pb = p % 2
                for t in tiles:
                    c = t // 4
                    outproj_add(g, lambda k, t=t: mixp[pb][:, t * 128:(t + 1) * 128], 1, WoC[pb], ("WoC", pb), t, pY,
                                [("Cmix%d" % pb, c, 0), ("Cmix%d" % pb, c, 1)])
                    yield

            def attn(p, filler=None):
                pb = p % 2
                own = (lambda c: outproj_gen(p, range(4 * c, 4 * c + 4))) if p == 3 else None
                attn_pair(g, "C", lambda hh, q0, N: qkT[pb][0:96, hh, q0:q0 + N],
                          lambda hh, j: qkT[pb][0:96, 2 + hh, j * 128:(j + 1) * 128],
                          lambda hh, j: vaug[pb][:, j, hh, :],
                          [("CqkT", pb), ("Cv", pb), ("vones", pb)], scale, "C", mixp[pb], "Cmix%d" % pb, pS, pO, E, P, rden, None,
                          mask_eng=("dve",), filler=filler, own_outproj=own)

            issue_w(0)
            issue_w(1)
            issue_wo(0)
            for _ in proj(0):
                pass
            og = iter(())
            for p in range(4):
                gen = proj(p + 1) if p + 1 < 4 else iter(())
                if p + 2 < 4:
                    issue_w(p + 2)
                st_ = {"i": 0}

                def filler(gen=gen, og=og, st_=st_):
                    st_["i"] += 1
                    next(gen, None)
                    if st_["i"] % 8 == 0:
                        next(og, None)

                attn(p, filler=filler)
                for _ in gen:
                    pass
                for _ in og:
                    pass
                if p + 1 < 4:
                    issue_wo(p + 1)
                og = outproj_gen(p) if p < 3 else iter(())
            R.emit_phase()


def phase_D(g, i):
    R = g.R
    HT, X = g.HT, g.X
    w_in = g.W['od_w_in'][i].rearrange("(c p) n -> p c n", p=128)
    w_out = g.W['od_w_out'][i]
    with ExitStack() as st:
        WD = R.sb("D_W", [128, 8, 256], BF16, st)
        WoD = R.sb("D_Wo", [128, 1, D], BF16, st)
        Wbd2 = [R.sb("D_Wbd%d" % b, [128, 2, 128], BF16, st) for b in range(2)]
        cols2 = [R.sb("D_cols%d" % b, [128, 12], F32, st) for b in range(2)]
        xrS2 = [R.sb("D_xr%d" % b, [128, S + 4], F32, st) for b in range(2)]
        xc2 = [R.sb("D_xc%d" % b, [128, S], F32, st) for b in range(2)]
        av2 = [R.sb("D_a%d" % b, [128, S], F32, st) for b in range(2)]
        uv2 = [R.sb("D_u%d" % b, [128, S], F32, st) for b in range(2)]
        gl2 = [R.sb("D_gl%d" % b, [128, S], BF16, st) for b in range(2)]
        xcb = R.sb("D_xcb", [128, S], BF16, st)
        mixD = R.sb("D_mix", [128, S], BF16, st)
        rr = R.sb("D_rr", [128, 512], F32, st)
        sq = R.sb("D_sq", [128, 512], F32, st)
        ig = R.sb("D_ig", [128, 512], F32, st)
        pxr = [R.ps("D_pxr%d" % b, [128, 512], F32, st) for b in range(2)]
        pgt = [R.ps("D_pgt%d" % b, [128, 512], F32, st) for b in range(2)]
        pa = R.ps("D_pa", [128, 512], F32, st)
        px = R.ps("D_px", [128, 512], F32, st)
        pY = [R.ps("D_pY", [128, 512], F32, st)]
        for b in range(2):
            R.op("dve", lambda e, b=b: e.memset(xrS2[b][:, 0:3], 0.0), writes=[("xpad", b)])

        def stage_a(ct):
            pb = ct % 2
            Wbd, cols, xrS, gl = Wbd2[pb], cols2[pb], xrS2[pb], gl2[pb]
            ch = slice(ct * 128, (ct + 1) * 128)
            wdma(g, WD[:, :, 0:128], w_in[:, :, 416 + ct * 128:416 + (ct + 1) * 128], "wd0", ("WD", 0))
            wdma(g, WD[:, :, 128:256], w_in[:, :, 928 + ct * 128:928 + (ct + 1) * 128], "wd1", ("WD", 1))
            R.op("pool", lambda e: e.memset(Wbd[:], 0.0), writes=[("Wbd", pb)])
            for s_, nm in enumerate(('rg_w_a', 'rg_w_x')):
                for hb_ in range(2):
                    R.dma("pool", lambda e, s_=s_, nm=nm, hb_=hb_: e.dma_start(
                        out=Wbd[hb_ * 64:(hb_ + 1) * 64, s_, hb_ * 64:(hb_ + 1) * 64], in_=g.W[nm][i, 2 * ct + hb_]),
                        "wbd%d%d%d" % (pb, s_, hb_), reads=[("Wbd", pb)], writes=[("Wbdb", pb, s_, hb_)])
            srcs = [g.W['rg_conv_w'][i, j:j + 1, ch] for j in range(4)] + [g.W[nm][i:i + 1, ch] for nm in ('rg_conv_b', 'rg_b_a', 'rg_b_x', 'rg_lambda')]
            for k_, src in enumerate(srcs):
                R.dma("sp", lambda e, k_=k_, src=src: e.dma_start(out=cols[:, k_:k_ + 1], in_=src.rearrange("o p -> p o")), "dcol%d_%d" % (pb, k_),
                      writes=[("cols", pb, k_)])
            R.op("act", lambda e: e.activation(out=cols[:, 8:9], in_=cols[:, 7:8], func=AF.Exp, scale=-1.0), reads=[("cols", pb, 7)], writes=[("cols", pb, 8)])
            R.op("act", lambda e: e.activation(out=cols[:, 8:9], in_=cols[:, 8:9], func=AF.Ln, bias=1.0), reads=[("cols", pb, 8)], writes=[("cols", pb, 8)])
            R.op("dve", lambda e: e.tensor_scalar(out=cols[:, 9:10], in0=cols[:, 8:9], scalar1=-16.0, scalar2=None, op0=ALU.mult),
                 reads=[("cols", pb, 8)], writes=[("cols", pb, 9)])
            R.op("dve", lambda e: e.tensor_scalar(out=cols[:, 8:9], in0=cols[:, 8:9], scalar1=-8.0, scalar2=None, op0=ALU.mult),
                 reads=[("cols", pb, 8), ("cols", pb, 9)], writes=[("cols", pb, 8)])
            for tg in range(4):
                b = tg % 2
                toks = slice(tg * 512, (tg + 1) * 512)
                hkeys = [("HT", t) for t in range(4 * tg, 4 * tg + 4)]
                for (pt, lo, nm, wk) in ((pxr, 0, "pxr", 0), (pgt, 128, "pgt", 1)):
                    for c in range(8):
                        R.op("pe", lambda e, pt=pt, lo=lo, c=c, b=b, toks=toks: e.matmul(pt[b][:], lhsT=WD[:, c, lo:lo + 128], rhs=HT[:, c, toks],
                                                                                       start=(c == 0), stop=(c == 7)),
                             reads=hkeys + [("WD", wk)], writes=[(nm, b)])
                R.op("act", lambda e, b=b, tg=tg: e.activation(out=xrS[:, 3 + tg * 512:3 + (tg + 1) * 512], in_=pxr[b][:], func=AF.Copy),
                     reads=[("pxr", b)], writes=[("xr", pb, tg)])
                R.op("act", lambda e, b=b, toks=toks: e.activation(out=gl[:, toks], in_=pgt[b][:], func=AF.Gelu_apprx_tanh), reads=[("pgt", b)], writes=[("gl", pb, tg)])

        def stage_b(ct):
            pb = ct % 2
            Wbd, cols, xrS, xc, av, uv = Wbd2[pb], cols2[pb], xrS2[pb], xc2[pb], av2[pb], uv2[pb]
            bdkeys = [("Wbdb", pb, s_, hb_) for s_ in range(2) for hb_ in range(2)]
            xrk = [("xr", pb, tg) for tg in range(4)] + [("xpad", pb)]
            R.op("dve", lambda e: e.tensor_scalar(out=xc[:], in0=xrS[:, 0:S], scalar1=cols[:, 0:1], scalar2=cols[:, 4:5], op0=ALU.mult, op1=ALU.add),
                 reads=xrk + [("cols", pb, 0), ("cols", pb, 4)], writes=[("xc", pb)])
            for j in range(1, 4):
                R.op("dve", lambda e, j=j: e.scalar_tensor_tensor(out=xc[:], in0=xrS[:, j:j + S], scalar=cols[:, j:j + 1], in1=xc[:], op0=ALU.mult, op1=ALU.add),
                     reads=xrk + [("cols", pb, j), ("xc", pb)], writes=[("xc", pb)])
            R.op("act", lambda e: e.activation(out=xcb[:], in_=xc[:], func=AF.Copy), reads=[("xc", pb)], writes=["xcb"])
            for tg in range(4):
                toks = slice(tg * 512, (tg + 1) * 512)
                R.op("pe", lambda e, toks=toks: e.matmul(pa[:], lhsT=Wbd[:, 0, :], rhs=xcb[:, toks], start=True, stop=True), reads=["xcb", ("Wbd", pb)] + bdkeys, writes=["pa"])
                R.op("pe", lambda e, toks=toks: e.matmul(px[:], lhsT=Wbd[:, 1, :], rhs=xcb[:, toks], start=True, stop=True), reads=["xcb", ("Wbd", pb)] + bdkeys, writes=["px"])
                R.op("act", lambda e: e.activation(out=rr[:], in_=pa[:], func=AF.Sigmoid, bias=cols[:, 5:6]), reads=["pa", ("cols", pb, 5)], writes=["rr"])
                R.op("act", lambda e: e.activation(out=ig[:], in_=px[:], func=AF.Sigmoid, bias=cols[:, 6:7]), reads=["px", ("cols", pb, 6)], writes=["ig"])
                R.op("act", lambda e, toks=toks: e.activation(out=av[:, toks], in_=rr[:], func=AF.Exp, scale=cols[:, 8:9]), reads=["rr", ("cols", pb, 8)], writes=[("av", pb, tg)])
                R.op("act", lambda e: e.activation(out=sq[:], in_=rr[:], func=AF.Exp, scale=cols[:, 9:10]), reads=["rr", ("cols", pb, 9)], writes=["sq"])
                R.op("act", lambda e: e.activation(out=sq[:], in_=sq[:], func=AF.Sqrt, scale=-1.0, bias=1.0), reads=["sq"], writes=["sq"])
                R.op("dve", lambda e, toks=toks: e.tensor_tensor(out=uv[:, toks], in0=ig[:], in1=xc[:, toks], op=ALU.mult), reads=["ig", ("xc", pb)], writes=[("uv", pb, tg)])
                R.op("dve", lambda e, toks=toks: e.tensor_tensor(out=uv[:, toks], in0=uv[:, toks], in1=sq[:], op=ALU.mult), reads=[("uv", pb, tg), "sq"], writes=[("uv", pb, tg)])

        def stage_c(ct):
            pb = ct % 2
            xrS, av, uv, gl = xrS2[pb], av2[pb], uv2[pb], gl2[pb]
            xrk = [("xr", pb, tg) for tg in range(4)] + [("xpad", pb)]
            wdma(g, WoD[:, 0, :], w_out[512 + ct * 128:512 + (ct + 1) * 128, :], "wod", "WoD")
            R.op("dve", lambda e: e.tensor_tensor_scan(out=xrS[:, 4:4 + S], data0=av[:], data1=uv[:], initial=0.0, op0=ALU.mult, op1=ALU.add),
                 reads=[("av", pb, tg) for tg in range(4)] + [("uv", pb, tg) for tg in range(4)] + xrk, writes=[("hsc", pb)] + [("xr", pb, tg) for tg in range(4)])
            R.op("dve", lambda e: e.tensor_tensor(out=mixD[:], in0=xrS[:, 4:4 + S], in1=gl[:], op=ALU.mult),
                 reads=[("hsc", pb)] + [("gl", pb, tg) for tg in range(4)], writes=["mixD"])
            for t in range(NT):
                outproj_add(g, lambda k, t=t: mixD[:, t * 128:(t + 1) * 128], 1, WoD, "WoD", t, pY, ["mixD"])

        stage_a(0)
        stage_b(0)
        for ct in range(4):
            if ct + 1 < 4:
                stage_a(ct + 1)
            stage_c(ct)
            if ct + 1 < 4:
                stage_b(ct + 1)
        R.emit_phase()


_NC_CACHE = {}


def _get_nc(key, **kw):
    if key not in _NC_CACHE:
        _NC_CACHE[key] = build(**kw)
    return _NC_CACHE[key]


def _in_maps(inputs, n):
    shared = {}
    for name, shp in IN_SHAPES.items():
        shared[name] = np.ascontiguousarray(np.asarray(inputs[name], dtype=np.float32).reshape(shp))
    x = np.asarray(inputs['x'], dtype=np.float32)
    maps = []
    for b in range(n):
        m = dict(shared)
        m['x'] = np.ascontiguousarray(x[b])
        maps.append(m)
    return maps


def kernel(**inputs):
    from concourse.bass_utils import run_bass_kernel_spmd
    n = 8
    nc = _get_nc("full")
    res = run_bass_kernel_spmd(nc, _in_maps(inputs, n), core_ids=list(range(n)))
    return np.stack([np.asarray(r["out"], dtype=np.float32) for r in res.results], axis=0)
```

```python
import numpy as np
import concourse.bass as bass
import concourse.mybir as mybir
from contextlib import ExitStack

F32 = mybir.dt.float32
BF16 = mybir.dt.bfloat16
I32 = mybir.dt.int32
U32 = mybir.dt.uint32
AF = mybir.ActivationFunctionType
ALU = mybir.AluOpType
AX = mybir.AxisListType

ENGS = ("pe", "act", "dve", "pool", "sp")


class Rec:
    def __init__(self, nc):
        self.nc = nc
        self.stack = ExitStack()
        self.sems = {e: self.stack.enter_context(nc.semaphore("e_" + e)) for e in ENGS}
        self.base = {e: 0 for e in ENGS}
        self.dma_sems = {}
        self.dma_touched = set()
        self.prev_barrier = None
        self.final_tokens = []
        self.n_instr = 0
        self._reset()

    def _reset(self):
        self.ops = {e: [] for e in ENGS}
        self.lastw = {}
        self.readers = {}

    def sb(self, name, shape, dt, stack=None):
        self.uid = getattr(self, "uid", 0) + 1
        return (stack or self.stack).enter_context(self.nc.sbuf_tensor("%s_%d" % (name, self.uid), list(shape), dt))

    def ps(self, name, shape, dt=F32, stack=None):
        self.uid = getattr(self, "uid", 0) + 1
        return (stack or self.stack).enter_context(self.nc.psum_tensor("%s_%d" % (name, self.uid), list(shape), dt))

    def _deps(self, reads, writes):
        deps = set()
        for k in reads:
            t = self.lastw.get(k)
            if t is not None:
                deps.add(t)
        for k in writes:
            t = self.lastw.get(k)
            if t is not None:
                deps.add(t)
            for r in self.readers.get(k, ()):
                deps.add(r)
        return deps

    def _commit(self, tok, reads, writes):
        for k in reads:
            self.readers.setdefault(k, []).append(tok)
        for k in writes:
            self.lastw[k] = tok
            self.readers[k] = []

    PSUM_KEYS = {"ptrn", "Ap", "Aptr", "Aptrb", "pl", "pmT", "pS", "pO", "pY", "pp", "Bptr", "pG", "pU", "pc", "Cptr",
                 "pq", "pkv", "Cptr2", "pxr", "pgt", "pa", "px"}

    def op(self, eng, fn, reads=(), writes=()):
        ex = [k for k in reads if (k[0] if isinstance(k, tuple) else k) in self.PSUM_KEYS]
        if ex:
            reads = [k for k in reads if k not in ex]
            writes = list(writes) + ex
        deps = self._deps(reads, writes)
        idx = len(self.ops[eng])
        tok = ("E", eng, idx)
        self.ops[eng].append(dict(fn=fn, deps=deps, signal=False, dma=None))
        self._commit(tok, reads, writes)
        return tok

    def dma(self, eng, fn, sem, reads=(), writes=(), final=False):
        deps = self._deps(reads, writes)
        if sem not in self.dma_sems:
            h = self.stack.enter_context(self.nc.semaphore("d_" + sem))
            self.dma_sems[sem] = [h, 0]
        ent = self.dma_sems[sem]
        ent[1] += 16
        self.dma_touched.add(sem)
        tok = ("D", sem, ent[1])
        if ent[1] > 16:
            deps.add(("D", sem, ent[1] - 16))
        self.ops[eng].append(dict(fn=fn, deps=deps, signal=False, dma=sem))
        self._commit(tok, reads, writes)
        if final:
            self.final_tokens.append(tok)
        return tok

    def emit_phase(self, last=False):
        nc = self.nc
        ops = self.ops
        sems = self.sems
        dma_sems = self.dma_sems
        for e in ENGS:
            for o in ops[e]:
                for d in o["deps"]:
                    if d[0] == "E":
                        if d[1] == "pe" and e == "pe":
                            continue
                        ops[d[1]][d[2]]["signal"] = True
            for o in reversed(ops[e]):
                if o["dma"] is None:
                    o["signal"] = True
                    break
        sigval = {}
        for e in ENGS:
            c = self.base[e]
            v = []
            for o in ops[e]:
                if o["signal"]:
                    c += 1
                v.append(c)
            sigval[e] = v
        new_base = {e: (sigval[e][-1] if sigval[e] else self.base[e]) for e in ENGS}
        prev = self.prev_barrier
        final_tokens = self.final_tokens if last else []

        def run(e, engobj):
            waited = {}
            if prev is not None:
                for e2, val in prev[0].items():
                    if e2 != e and val > 0:
                        engobj.wait_ge(sems[e2], val)
                        waited[("E", e2)] = val
                for s, val in prev[1].items():
                    engobj.wait_ge(dma_sems[s][0], val)
                    waited[("D", s)] = val
            for o in ops[e]:
                need = {}
                for d in o["deps"]:
                    if d[0] == "E":
                        if d[1] == "pe" and e == "pe":
                            continue
                        key = ("E", d[1])
                        val = sigval[d[1]][d[2]]
                    else:
                        key = ("D", d[1])
                        val = d[2]
                    if val > need.get(key, 0):
                        need[key] = val
                for key, val in need.items():
                    if waited.get(key, 0) >= val:
                        continue
                    waited[key] = val
                    if key[0] == "E":
                        engobj.wait_ge(sems[key[1]], val)
                    else:
                        engobj.wait_ge(dma_sems[key[1]][0], val)
                ins = o["fn"](engobj)
                self.n_instr += 1
                if o["dma"] is not None:
                    ins.then_inc(dma_sems[o["dma"]][0], 16)
                elif o["signal"]:
                    ins.then_inc(sems[e], 1)
            if last and e == "sp":
                for t in final_tokens:
                    engobj.wait_ge(dma_sems[t[1]][0], t[2])

        with nc.Block() as block:
            @block.tensor
            def _(eng):
                run("pe", eng)

            @block.scalar
            def _(eng):
                run("act", eng)

            @block.vector
            def _(eng):
                run("dve", eng)

            @block.gpsimd
            def _(eng):
                run("pool", eng)

            @block.sync
            def _(eng):
                run("sp", eng)

        self.base = new_base
        self.prev_barrier = (dict(new_base), {s: dma_sems[s][1] for s in self.dma_touched})
        self.dma_touched = set()
        self._reset()

    def close(self):
        self.stack.close()


S = 2048
D = 1024
NT = 16
DFF = 3584
NSLAB = 7
EPS = 1e-6
THETA = 500000.0
NEG = -1.0e30
import math

IN_SHAPES = {
    'ev_norm_mix': (2, 1024), 'ev_w_in': (2, 1024, 2600), 'ev_w_out': (2, 1024, 1024), 'ev_norm_ffn': (2, 1024),
    'ffn_w_gate': (2, 1024, 3584), 'ffn_w_up': (2, 1024, 3584), 'ffn_w_down': (2, 3584, 1024),
    'od_norm_mix': (2, 1024), 'od_w_in': (2, 1024, 1440), 'mla_q_norm': (2, 256), 'mla_w_uq': (2, 256, 768),
    'mla_kv_norm': (2, 128), 'mla_w_ukv': (2, 128, 1024), 'rg_conv_w': (2, 4, 512), 'rg_conv_b': (2, 512),
    'rg_w_a': (2, 8, 64, 64), 'rg_b_a': (2, 512), 'rg_w_x': (2, 8, 64, 64), 'rg_b_x': (2, 512), 'rg_lambda': (2, 512),
    'od_w_out': (2, 1024, 1024), 'od_norm_ffn': (2, 1024), 'moe_router': (2, 1024, 8),
    'moe_w_gate': (2, 8, 1024, 3584), 'moe_w_up': (2, 8, 1024, 3584), 'moe_w_down': (2, 8, 3584, 1024),
    'final_norm': (1, 1024),
}


class K:
    pass


def build(layers=(0, 1, 2, 3), do_final=True, stop=None, n_experts=8):
    nc = bass.Bass("TRN2", target_bir_lowering=False)
    g = K()
    g.nc = nc
    g.W = {}
    g.x_in = nc.dram_tensor("x", [S, D], F32, kind="ExternalInput").ap()
    for name, shp in IN_SHAPES.items():
        g.W[name] = nc.dram_tensor(name, list(shp), F32, kind="ExternalInput").ap()
    g.out = nc.dram_tensor("out", [S, D], F32, kind="ExternalOutput").ap()
    R = Rec(nc)
    g.R = R
    g.X = R.sb("X", [128, NT, D], F32)
    g.HT = R.sb("HT", [128, 8, S], BF16)
    g.ident = R.sb("ident", [128, 128], BF16)
    g.gbc = R.sb("gbc", [128, D], F32)
    g.ss = R.sb("ss", [128, NT], F32)
    g.rstd = R.sb("rstd", [128, NT], F32)
    g.TB = R.sb("TB", [128, S], BF16)
    g.TC = R.sb("TC", [128, 512], BF16)
    g.NTRI = R.sb("NTRI", [128, 128], F32)
    g.rope = {}
    for rot in (16, 8, 32):
        g.rope[rot] = (R.sb("cos%d" % rot, [128, NT, rot // 2], F32), R.sb("sin%d" % rot, [128, NT, rot // 2], F32))
    g.n_experts = n_experts

    first = layers[0] if len(layers) else None
    first_gain = None
    if first is not None:
        first_gain = (g.W['ev_norm_mix'] if first % 2 == 0 else g.W['od_norm_mix'])[first // 2:first // 2 + 1, :]
    phase_init(g, first_gain)
    merged_final = False
    pre_normed = True
    for li, L in enumerate(layers):
        i = L // 2
        is_last = (L == layers[-1]) and stop is None
        nxt = layers[li + 1] if li + 1 < len(layers) else None
        nxt_gain = None
        if nxt is not None and stop is None:
            nxt_gain = (g.W['ev_norm_mix'] if nxt % 2 == 0 else g.W['od_norm_mix'])[nxt // 2:nxt // 2 + 1, :]
        ntail = (lambda st, ng=nxt_gain: phase_norm(g, ng, st=st, reuse=True)) if nxt_gain is not None else None
        if L % 2 == 0:
            if not pre_normed:
                phase_norm(g, g.W['ev_norm_mix'][i:i + 1, :])
            phase_A(g, i)
            phase_B(g, i)
            if stop == "mix":
                break
            phase_ffn(g, g.W['ffn_w_gate'][i], g.W['ffn_w_up'][i], g.W['ffn_w_down'][i], None, norm_gain=g.W['ev_norm_ffn'][i:i + 1, :], tail=ntail)
            pre_normed = ntail is not None
        else:
            phase_C(g, i, norm_gain=(None if pre_normed else g.W['od_norm_mix'][i:i + 1, :]))
            phase_D(g, i)
            if stop == "mix":
                break
            tail = (lambda st: phase_final(g, do_final, st=st)) if is_last else None
            phase_moe(g, i, norm_gain=g.W['od_norm_ffn'][i:i + 1, :], tail=tail, ntail=ntail)
            merged_final = merged_final or is_last
            pre_normed = ntail is not None
    if not merged_final:
        phase_final(g, do_final)
    R.close()
    return nc


def phase_init(g, first_gain=None):
    R = g.R
    X = g.X
    xin = g.x_in.rearrange("(t p) d -> p t d", p=128)
    for t in range(NT):
        R.dma("sp" if t % 2 == 0 else "act", lambda e, t=t: e.dma_start(out=X[:, t, :], in_=xin[:, t, :]),
              "xin%d" % t, writes=[("X", t, 0), ("X", t, 1)])
    with ExitStack() as st:
        Di = R.sb("Di", [128, S], I32, st)
        Df = R.sb("Df", [128, S], F32, st)
        Ai = R.sb("Ai", [128, S], I32, st)
        m0 = R.sb("m0", [128, S], F32, st)
        m1 = R.sb("m1", [128, S], F32, st)
        m2 = R.sb("m2", [128, S], F32, st)
        ident = g.ident
        R.op("pool", lambda e: e.memset(ident[:], 0.0), writes=["ident"])
        R.op("pool", lambda e: e.affine_select(out=ident[:], in_=ident[:], pattern=[[-1, 128]], compare_op=ALU.not_equal,
                                               fill=1.0, base=0, channel_multiplier=1), reads=["ident"], writes=["ident"])
        R.op("pool", lambda e: e.iota(Di[:], pattern=[[1, S]], base=0, channel_multiplier=-1), writes=["Di"])
        R.op("dve", lambda e: e.tensor_copy(out=Df[:], in_=Di[:]), reads=["Di"], writes=["Df"])
        R.op("dve", lambda e: e.tensor_scalar(out=m0[:], in0=Df[:], scalar1=0.0, scalar2=None, op0=ALU.is_ge), reads=["Df"], writes=["m0"])
        TC, NTRI, TB = g.TC, g.NTRI, g.TB
        R.op("dve", lambda e: e.tensor_copy(out=TC[:], in_=m0[:, 0:512]), reads=["m0"], writes=["TC"])
        R.op("dve", lambda e: e.tensor_scalar(out=NTRI[:], in0=Df[:, 0:128], scalar1=0.0, scalar2=2.0 * NEG, op0=ALU.is_gt, op1=ALU.mult),
             reads=["Df"], writes=["NTRI"])
        R.op("dve", lambda e: e.scalar_tensor_tensor(out=m1[:], in0=Df[:], scalar=128.0, in1=m0[:], op0=ALU.is_le, op1=ALU.mult),
             reads=["Df", "m0"], writes=["m1"])
        R.op("dve", lambda e: e.tensor_single_scalar(out=Ai[:], in_=Di[:], scalar=3, op=ALU.bitwise_and), reads=["Di"], writes=["Ai"])
        R.op("dve", lambda e: e.tensor_copy(out=m2[:], in_=Ai[:]), reads=["Ai"], writes=["m2"])
        R.op("dve", lambda e: e.scalar_tensor_tensor(out=m2[:], in0=m2[:], scalar=0.0, in1=m0[:], op0=ALU.is_equal, op1=ALU.mult),
             reads=["m2", "m0"], writes=["m2"])
        R.op("dve", lambda e: e.scalar_tensor_tensor(out=m2[:], in0=Df[:], scalar=512.0, in1=m2[:], op0=ALU.is_le, op1=ALU.mult),
             reads=["m2", "Df"], writes=["m2"])
        R.op("dve", lambda e: e.tensor_tensor(out=m1[:], in0=m1[:], in1=m2[:], op=ALU.add), reads=["m1", "m2"], writes=["m1"])
        R.op("dve", lambda e: e.tensor_single_scalar(out=Ai[:], in_=Di[:], scalar=15, op=ALU.bitwise_and), reads=["Di"], writes=["Ai"])
        R.op("dve", lambda e: e.tensor_copy(out=m2[:], in_=Ai[:]), reads=["Ai"], writes=["m2"])
        R.op("dve", lambda e: e.scalar_tensor_tensor(out=m2[:], in0=m2[:], scalar=0.0, in1=m0[:], op0=ALU.is_equal, op1=ALU.mult),
             reads=["m2", "m0"], writes=["m2"])
        R.op("dve", lambda e: e.tensor_tensor(out=TB[:], in0=m1[:], in1=m2[:], op=ALU.add), reads=["m1", "m2"], writes=["TB"])
        posi = R.sb("posi", [128, NT], I32, st)
        posf = R.sb("posf", [128, NT], F32, st)
        R.op("pool", lambda e: e.iota(posi[:], pattern=[[128, NT]], base=0, channel_multiplier=1), writes=["posi"])
        R.op("dve", lambda e: e.tensor_copy(out=posf[:], in_=posi[:]), reads=["posi"], writes=["posf"])
        for rot in (16, 8, 32):
            nf = rot // 2
            fi = R.sb("fi%d" % rot, [128, nf], I32, st)
            ff = R.sb("ff%d" % rot, [128, nf], F32, st)
            ang = R.sb("ang%d" % rot, [128, NT, nf], F32, st)
            ang2 = R.sb("ang2%d" % rot, [128, NT, nf], F32, st)
            ki = R.sb("ki%d" % rot, [128, NT, nf], I32, st)
            kf = R.sb("kf%d" % rot, [128, NT, nf], F32, st)
            cs, sn = g.rope[rot]
            k0 = "r%d" % rot
            R.op("pool", lambda e, fi=fi, nf=nf: e.iota(fi[:], pattern=[[1, nf]], base=0, channel_multiplier=0), writes=[k0 + "fi"])
            R.op("dve", lambda e, fi=fi, ff=ff: e.tensor_copy(out=ff[:], in_=fi[:]), reads=[k0 + "fi"], writes=[k0 + "ff"])
            R.op("act", lambda e, ff=ff, rot=rot: e.activation(out=ff[:], in_=ff[:], func=AF.Exp, scale=-math.log(THETA) * 2.0 / rot),
                 reads=[k0 + "ff"], writes=[k0 + "ff"])
            R.op("dve", lambda e, ang=ang, ff=ff, nf=nf: e.tensor_tensor(
                out=ang[:], in0=posf[:].unsqueeze(2).to_broadcast([128, NT, nf]),
                in1=ff[:].unsqueeze(1).to_broadcast([128, NT, nf]), op=ALU.mult), reads=["posf", k0 + "ff"], writes=[k0 + "ang"])
            for which, dst, shift in (("s", sn, 0.0), ("c", cs, math.pi / 2)):
                kk = k0 + which
                R.op("dve", lambda e, ang=ang, ang2=ang2, shift=shift: e.tensor_scalar(
                    out=ang2[:], in0=ang[:], scalar1=shift, scalar2=None, op0=ALU.add), reads=[k0 + "ang"], writes=[k0 + "ang2"])
                R.op("dve", lambda e, ang2=ang2, ki=ki: e.tensor_scalar(
                    out=ki[:], in0=ang2[:], scalar1=1.0 / (2 * math.pi), scalar2=None, op0=ALU.mult), reads=[k0 + "ang2"], writes=[k0 + "ki"])
                R.op("dve", lambda e, kf=kf, ki=ki: e.tensor_copy(out=kf[:], in_=ki[:]), reads=[k0 + "ki"], writes=[k0 + "kf"])
                R.op("dve", lambda e, kf=kf, ang2=ang2: e.scalar_tensor_tensor(
                    out=ang2[:], in0=kf[:], scalar=-2 * math.pi, in1=ang2[:], op0=ALU.mult, op1=ALU.add),
                    reads=[k0 + "kf", k0 + "ang2"], writes=[k0 + "ang2"])
                R.op("dve", lambda e, ang2=ang2: e.tensor_scalar(
                    out=ang2[:], in0=ang2[:], scalar1=3.14159, scalar2=-3.14159, op0=ALU.min, op1=ALU.max),
                    reads=[k0 + "ang2"], writes=[k0 + "ang2"])
                R.op("act", lambda e, ang2=ang2, dst=dst: e.activation(out=dst[:], in_=ang2[:], func=AF.Sin),
                     reads=[k0 + "ang2"], writes=[kk + "tab"])
        if first_gain is not None:
            phase_norm(g, first_gain, st=st)
        R.emit_phase()


def phase_norm(g, gain_row, emit=True, st=None, reuse=False):
    R = g.R
    X, HT, gbc, ss, rstd, ident = g.X, g.HT, g.gbc, g.ss, g.rstd, g.ident
    own = ExitStack() if st is None else None
    if st is not None:
        emit = False
    with (own if own is not None else ExitStack()) as st_:
        st = st_ if own is not None else st
        if reuse:
            junk, hb, ptr = g._norm_bufs
        else:
            junk = R.sb("junk", [128, D], BF16, st)
            hb = [R.sb("hb%d" % b, [128, D], BF16, st) for b in range(2)]
            ptr = [R.ps("ptrn%d" % b, [128, 8, 128], BF16, st) for b in range(2)]
            g._norm_bufs = (junk, hb, ptr)
        R.dma("sp", lambda e: e.dma_start(out=gbc[:], in_=gain_row.to_broadcast([128, D])), "gbc", writes=["gbc"])
        for t in range(NT):
            R.op("act", lambda e, t=t: e.activation(out=junk[:], in_=X[:, t, :], func=AF.Square, accum_out=ss[:, t:t + 1]),
                 reads=[("X", t, 0), ("X", t, 1)], writes=["junk", ("ss", t)])
        allss = [("ss", t) for t in range(NT)]
        R.op("dve", lambda e: e.tensor_scalar(out=rstd[:], in0=ss[:], scalar1=1.0 / D, scalar2=EPS, op0=ALU.mult, op1=ALU.add),
             reads=allss, writes=["rstd"])
        R.op("act", lambda e: e.activation(out=rstd[:], in_=rstd[:], func=AF.Sqrt), reads=["rstd"], writes=["rstd"])
        R.op("dve", lambda e: e.reciprocal(out=rstd[:], in_=rstd[:]), reads=["rstd"], writes=["rstd"])
        for t in range(NT):
            b = t % 2
            R.op("dve", lambda e, t=t, b=b: e.scalar_tensor_tensor(out=hb[b][:], in0=X[:, t, :], scalar=rstd[:, t:t + 1], in1=gbc[:],
                                                                   op0=ALU.mult, op1=ALU.mult),
                 reads=[("X", t, 0), ("X", t, 1), "rstd", "gbc"], writes=[("hb", b)])
            for c in range(8):
                R.op("pe", lambda e, b=b, c=c: e.transpose(out=ptr[b][:, c, :], in_=hb[b][:, c * 128:(c + 1) * 128], identity=ident[:]),
                     reads=[("hb", b), "ident"], writes=[("ptrn", b)])
            R.op("act", lambda e, t=t, b=b: e.activation(out=HT[:, :, t * 128:(t + 1) * 128], in_=ptr[b][:], func=AF.Copy),
                 reads=[("ptrn", b)], writes=[("HT", t)])
        if emit:
            R.emit_phase()


def rope_tok(g, src, dst, nh, off, rot, t, tmp, rkeys, wkeys):
    R = g.R
    half = rot // 2
    cs, sn = g.rope[rot]
    cb = cs[:, t, :].unsqueeze(1).to_broadcast([128, nh, half])
    sb_ = sn[:, t, :].unsqueeze(1).to_broadcast([128, nh, half])
    x1 = src[:, :, off:off + half]
    x2 = src[:, :, off + half:off + rot]
    t1 = tmp[0][:, 0:nh * half].rearrange("p (h f) -> p h f", h=nh)
    t2 = tmp[1][:, 0:nh * half].rearrange("p (h f) -> p h f", h=nh)
    k1, k2 = ("rt", 0), ("rt", 1)
    R.op("dve", lambda e: e.tensor_tensor(out=t1, in0=x1, in1=cb, op=ALU.mult), reads=rkeys, writes=[k1])
    R.op("dve", lambda e: e.tensor_tensor(out=t2, in0=x2, in1=sb_, op=ALU.mult), reads=rkeys, writes=[k2])
    R.op("dve", lambda e: e.tensor_tensor(out=dst[:, :, off:off + half], in0=t1, in1=t2, op=ALU.subtract), reads=[k1, k2], writes=wkeys)
    R.op("dve", lambda e: e.tensor_tensor(out=t1, in0=x1, in1=sb_, op=ALU.mult), reads=rkeys, writes=[k1])
    R.op("dve", lambda e: e.tensor_tensor(out=t2, in0=x2, in1=cb, op=ALU.mult), reads=rkeys, writes=[k2])
    R.op("dve", lambda e: e.tensor_tensor(out=dst[:, :, off + half:off + rot], in0=t1, in1=t2, op=ALU.add), reads=[k1, k2], writes=wkeys)


def wdma(g, dst, src, sem, key):
    g.R.dma("pool", lambda e: e.dma_start(out=dst, in_=src), sem, writes=[key])


def outproj_add(g, lhs_fn, nk, Wo, wkey, t, pY, mixkeys, pykey="pY"):
    R = g.R
    X = g.X
    for half in range(2):
        b = (t * 2 + half) % len(pY)
        for k in range(nk):
            R.op("pe", lambda e, k=k, b=b, half=half: e.matmul(pY[b][:], lhsT=lhs_fn(k), rhs=Wo[:, k, half * 512:(half + 1) * 512],
                                                            start=(k == 0), stop=(k == nk - 1)),
                 reads=mixkeys + [wkey], writes=[(pykey, b)])
        R.op("dve", lambda e, b=b, half=half: e.tensor_tensor(out=X[:, t, half * 512:(half + 1) * 512], in0=pY[b][:],
                                                              in1=X[:, t, half * 512:(half + 1) * 512], op=ALU.add),
             reads=[(pykey, b), ("X", t, half)], writes=[("X", t, half)])


def attn_pair(g, nm, q_fn, k_fn, v_fn, in_keys, scale, mode, mixp, mixkey, pS, pO, E, P, rden, after_chunk=None, mask_eng="pool", filler=None, own_outproj=None):
    R = g.R
    NB = len(pS)
    LOOK = NB - 1
    steps = []
    for c in range(4):
        for hh in range(2):
            nj = 4 * c + 4
            for j in range(nj):
                steps.append((c, hh, j, nj))
    deferred = []
    info = {}

    def emit_qk(s):
        c, hh, j, nj = steps[s]
        r = max(0, j - 4 * c)
        N = 512 - 128 * r
        q0 = (4 * c + r) * 128
        X0 = q0 - 128 * j
        b = s % NB
        R.op("pe", lambda e: e.matmul(pS[b][:, 0:N], lhsT=k_fn(hh, j), rhs=q_fn(hh, q0, N), start=True, stop=True),
             reads=in_keys, writes=[("pS", b)])
        R.op("act", lambda e: e.activation(out=E[b][:, 0:N], in_=pS[b][:, 0:N], func=AF.Exp, scale=scale),
             reads=[("pS", b)], writes=[("E", b)])
        if mode == "B" or X0 == 0:
            tab = g.TB[:, X0:X0 + N] if mode == "B" else g.TC[:, 0:N]
            eng = mask_eng if isinstance(mask_eng, str) else mask_eng[s % len(mask_eng)]
            R.op(eng, lambda e: e.tensor_tensor(out=P[b][:, 0:N], in0=E[b][:, 0:N], in1=tab, op=ALU.mult),
                 reads=[("E", b)], writes=[("P", b)])
            info[s] = (P[b], ("P", b), r, N)
        else:
            info[s] = (E[b], ("E", b), r, N)

    def emit_pv(s):
        c, hh, j, nj = steps[s]
        src, skey, r, N = info.pop(s)
        bo = (c * 2 + hh) % len(pO)
        R.op("pe", lambda e: e.matmul(pO[bo][:, 128 * r:512], lhsT=v_fn(hh, j), rhs=src[:, 0:N], start=(j == 0), stop=(j == nj - 1)),
             reads=[skey] + in_keys, writes=[("pO", bo)])
        if j == nj - 1:
            R.op("act", lambda e: e.activation(out=rden[64:128, :], in_=pO[bo][64:128, :], func=AF.Ln), reads=[("pO", bo)], writes=["rden"])
            R.op("act", lambda e: e.activation(out=rden[64:128, :], in_=rden[64:128, :], func=AF.Exp, scale=-1.0), reads=["rden"], writes=["rden"])
            R.op("dve", lambda e: e.tensor_tensor(out=mixp[hh * 64:(hh + 1) * 64, c * 512:(c + 1) * 512],
                                                  in0=pO[bo][0:64, :], in1=rden[64:128, :], op=ALU.mult),
                 reads=[("pO", bo), "rden"], writes=[(mixkey, c, hh)])
            if hh == 1 and after_chunk is not None:
                deferred.append((s + 4, lambda c=c: after_chunk(c)))
            if hh == 1 and own_outproj is not None:
                own.append([s + 4, own_outproj(c)])

    n = len(steps)
    own = []
    for s in range(n + LOOK):
        if s < n:
            emit_qk(s)
            if filler is not None:
                filler()
        if s - LOOK >= 0:
            emit_pv(s - LOOK)
            while deferred and deferred[0][0] <= s - LOOK:
                deferred.pop(0)[1]()
            if own and own[0][0] <= s - LOOK:
                if next(own[0][1], "end") == "end":
                    own.pop(0)
    while deferred:
        deferred.pop(0)[1]()
    for _, gen_ in own:
        for _ in gen_:
            pass


def phase_A(g, i):
    R = g.R
    HT, X, ident = g.HT, g.X, g.ident
    w_in = g.W['ev_w_in'][i]
    w_out = g.W['ev_w_out'][i]
    wi_scale = (8 ** -0.5) * (32 ** -0.5)
    with ExitStack() as st:
        qT = R.sb("A_qT", [128, NT, 4, 128], BF16, st)
        kT = R.sb("A_kT", [128, 2, S], BF16, st)
        vaug = R.sb("A_vaug", [128, NT, 3, 64], BF16, st)
        qiT = R.sb("A_qiT", [128, 3, S], BF16, st)
        kiT = R.sb("A_kiT", [128, S], BF16, st)
        wiS = R.sb("A_wiS", [128, NT, 8], F32, st)
        WoA = R.sb("A_Wo", [128, 4, D], BF16, st)
        with ExitStack() as st2:
            WA = R.sb("A_W", [128, 8, 1064], BF16, st2)
            stq = [R.sb("A_stq%d" % b, [128, 512], BF16, st2) for b in range(2)]
            stk = [R.sb("A_stk%d" % b, [128, 128], BF16, st2) for b in range(2)]
            sti = [R.sb("A_sti%d" % b, [128, 416], BF16, st2) for b in range(2)]
            tmp = [R.sb("A_rt%d" % b, [128, 128], F32, st2) for b in range(2)]
            p0 = [R.ps("A_p0%d" % b, [128, 512], F32, st2) for b in range(2)]
            p1 = [R.ps("A_p1", [128, 512], F32, st2)] * 2
            p2 = [R.ps("A_p2", [128, 512], F32, st2)] * 2
            ptrb = [R.ps("A_ptrb%d" % b, [128, 8, 128], BF16, st2) for b in range(2)]
            ptr = [R.ps("A_ptr%d" % b, [128, 8, 128], BF16, st2) for b in range(2)]
            win = w_in.rearrange("(c p) n -> p c n", p=128)
            wdma(g, WA[:, :, 0:512], win[:, :, 0:512], "wa0", ("WA", 0))
            wdma(g, WA[:, :, 512:768], win[:, :, 512:768], "wa1", ("WA", 1))
            wdma(g, WA[:, :, 768:1064], win[:, :, 768:1064], "wa2", ("WA", 2))
            wdma(g, WoA[:], w_out[0:512, :].rearrange("(c p) n -> p c n", p=128), "wa3", "WoA")
            R.op("dve", lambda e: e.memset(vaug[:, :, 1, :], 1.0), writes=["vones"])
            R.op("pool", lambda e: e.memset(kT[:], 0.0), writes=["kzero"])
            for t in range(NT):
                b = t % 2
                tok = slice(t * 128, (t + 1) * 128)
                for (pp, lo, hi, gi) in ((p0, 0, 512, 0), (p1, 512, 768, 1), (p2, 768, 1064, 2)):
                    for c in range(8):
                        R.op("pe", lambda e, pp=pp, lo=lo, hi=hi, c=c, b=b, tok=tok: e.matmul(
                            pp[b][:, 0:hi - lo], lhsT=HT[:, c, tok], rhs=WA[:, c, lo:hi], start=(c == 0), stop=(c == 7)),
                            reads=[("HT", t), ("WA", gi)], writes=[("Ap", gi, b if gi == 0 else 0)])
                R.op("act", lambda e, b=b: e.activation(out=stq[b][:].rearrange("p (g n d) -> p n g d", g=4, n=2),
                                                        in_=p0[b][:].rearrange("p (n g d) -> p n g d", n=2, g=4), func=AF.Copy),
                     reads=[("Ap", 0, b)], writes=[("stq", b)])
                for n_ in range(2):
                    rope_tok(g, p0[b][:, n_ * 256:(n_ + 1) * 256].rearrange("p (h d) -> p h d", h=4),
                             stq[b][:].rearrange("p (g n d) -> p g n d", g=4, n=2)[:, :, n_, :],
                             4, 0, 16, t, tmp, [("Ap", 0, b)], [("stq", b)])
                R.op("act", lambda e, b=b: e.activation(out=stk[b][:], in_=p1[b][:, 0:128], func=AF.Copy), reads=[("Ap", 1, 0)], writes=[("stk", b)])
                rope_tok(g, p1[b][:, 0:128].rearrange("p (h d) -> p h d", h=2), stk[b][:].rearrange("p (h d) -> p h d", h=2),
                         2, 0, 16, t, tmp, [("Ap", 1, 0)], [("stk", b)])
                for n_ in range(2):
                    R.op("act", lambda e, b=b, t=t, n_=n_: e.activation(out=vaug[:, t, 2 * n_, :], in_=p1[b][:, 128 + 64 * n_:192 + 64 * n_], func=AF.Copy),
                         reads=[("Ap", 1, 0)], writes=[("vaug", t)])
                R.op("act", lambda e, b=b: e.activation(out=sti[b][:, 0:288], in_=p2[b][:, 0:288], func=AF.Copy), reads=[("Ap", 2, 0)], writes=[("sti", b)])
                rope_tok(g, p2[b][:, 0:288].rearrange("p (h d) -> p h d", h=9), sti[b][:, 0:288].rearrange("p (h d) -> p h d", h=9),
                         9, 0, 8, t, tmp, [("Ap", 2, 0)], [("sti", b)])
                R.op("act", lambda e, b=b, t=t: e.activation(out=wiS[:, t, :], in_=p2[b][:, 288:296], func=AF.Copy, scale=wi_scale),
                     reads=[("Ap", 2, 0)], writes=[("wiS", t)])
                R.op("dve", lambda e, b=b: e.tensor_copy(out=sti[b][:, 288:384].rearrange("p (r d) -> p r d", r=3),
                                                        in_=sti[b][:, 256:288].unsqueeze(1).to_broadcast([128, 3, 32])),
                     reads=[("sti", b)], writes=[("sti4", b)])
                for gi in range(4):
                    R.op("pe", lambda e, b=b, gi=gi: e.transpose(
                        out=ptr[b][:, gi, :], in_=stq[b][:, gi * 128:(gi + 1) * 128], identity=ident[:]),
                        reads=[("stq", b), "ident"], writes=[("Aptr", b)])
                R.op("pe", lambda e, b=b: e.transpose(out=ptr[b][:, 4, :], in_=stk[b][:], identity=ident[:]), reads=[("stk", b)], writes=[("Aptr", b)])
                for hi_ in range(3):
                    wd_ = 96 if hi_ < 2 else 64
                    R.op("pe", lambda e, b=b, hi_=hi_, wd_=wd_: e.transpose(out=ptrb[b][0:wd_, hi_, :], in_=sti[b][:, hi_ * 96:hi_ * 96 + wd_], identity=ident[:]),
                         reads=[("sti", b)], writes=[("Aptrb", b)])
                R.op("pe", lambda e, b=b: e.transpose(out=ptrb[b][0:96, 3, :], in_=sti[b][:, 288:384], identity=ident[:]), reads=[("sti4", b)], writes=[("Aptrb", b)])
                R.op("act", lambda e, b=b, t=t: e.activation(out=qT[:, t, :, :], in_=ptr[b][:, 0:4, :], func=AF.Copy), reads=[("Aptr", b)], writes=[("AqT", t)])
                R.op("act", lambda e, b=b, tok=tok: e.activation(out=kT[0:64, 0, tok], in_=ptr[b][0:64, 4, :], func=AF.Copy), reads=[("Aptr", b), "kzero"], writes=[("AkT", t)])
                R.op("act", lambda e, b=b, tok=tok: e.activation(out=kT[64:128, 1, tok], in_=ptr[b][64:128, 4, :], func=AF.Copy), reads=[("Aptr", b), "kzero"], writes=[("AkT", t)])
                R.op("act", lambda e, b=b, tok=tok: e.activation(out=qiT[0:96, 0:2, tok], in_=ptrb[b][0:96, 0:2, :], func=AF.Copy), reads=[("Aptrb", b)], writes=[("AqiT", t)])
                R.op("act", lambda e, b=b, tok=tok: e.activation(out=qiT[0:64, 2, tok], in_=ptrb[b][0:64, 2, :], func=AF.Copy), reads=[("Aptrb", b)], writes=[("AqiT", t)])
                R.op("act", lambda e, b=b, tok=tok: e.activation(out=kiT[0:96, tok], in_=ptrb[b][0:96, 3, :], func=AF.Copy), reads=[("Aptrb", b)], writes=[("AkiT", t)])
            R.emit_phase()
        with ExitStack() as st2:
            acc = [R.sb("A_acc%d" % b, [128, S], F32, st2) for b in range(2)]
            junk = R.sb("A_junk", [128, S], mybir.dt.uint8, st2)
            junk2 = junk
            bs = R.sb("A_bs", [128, 2, 8], F32, st2)
            rl = [R.sb("A_rl%d" % b, [128, 512], F32, st2) for b in range(2)]
            KBIS = 28
            P2 = R.sb("A_P2", [128, KBIS], F32, st2)
            wk = R.sb("A_wk", [128, 2, KBIS], F32, st2)
            for k in range(KBIS):
                R.op("pool", lambda e, k=k: e.memset(P2[:, k:k + 1], 2.0 ** -(k + 1)), writes=["P2"])
            maskQ = [R.sb("A_mq%d" % b, [128, S], BF16, st2) for b in range(2)]
            maskT = R.sb("A_mT", [128, NT, 128], BF16, st2)
            E = [R.sb("A_E%d" % b, [128, 512], BF16, st2) for b in range(3)]
            qiz = R.sb("A_qiz", [128, 8, 128], BF16, st2)
            R.op("pool", lambda e: e.memset(qiz[:], 0.0), writes=["qiz0"])
            rden = R.sb("A_rden", [128, 512], F32, st2)
            mixA = [R.sb("A_mix", [128, 4, 128], BF16, st2)] * 2
            pl = [R.ps("A_pl%d" % b, [128, 512], F32, st2) for b in range(2)]
            pmT = R.ps("A_pmT", [128, 8, 128], BF16, st2)
            pS = [R.ps("A_pS%d" % b, [128, 512], F32, st2) for b in range(3)]
            pO = [R.ps("A_pO%d" % b, [128, 512], F32, st2) for b in range(2)]
            pY = pl
            cnt = {"pl": 0, "pS": 0}

            def idx_scores(qb):
                n = 128 * (qb + 1)
                ac = acc[qb % 2]
                ak = ("acc", qb % 2)
                qtok = slice(qb * 128, (qb + 1) * 128)
                for h in range(8):
                    hi_, hp = h // 3, h % 3
                    R.op("pool", lambda e, h=h, hi_=hi_, hp=hp: e.tensor_copy(out=qiz[32 * hp:32 * hp + 32, h, :], in_=qiT[32 * hp:32 * hp + 32, hi_, qtok]),
                         reads=[("AqiT", qb), "qiz0"], writes=[("qiz", h)])
                for kc in range((n + 511) // 512):
                    lo_, hi = kc * 512, min(n, (kc + 1) * 512)
                    w = hi - lo_
                    for h in range(8):
                        hi_, hp = h // 3, h % 3
                        b = cnt["pl"] % 2
                        cnt["pl"] += 1
                        R.op("pe", lambda e, w=w, lo_=lo_, hi=hi, h=h, b=b: e.matmul(
                            pl[b][:, 0:w], lhsT=qiz[0:96, h, :], rhs=kiT[0:96, lo_:hi], start=True, stop=True),
                            reads=[("qiz", h)] + [("AkiT", tt) for tt in range(lo_ // 128, hi // 128)], writes=[("pl", b)])
                        R.op("act", lambda e, b=b, w=w: e.activation(out=rl[b][:, 0:w], in_=pl[b][:, 0:w], func=AF.Relu), reads=[("pl", b)], writes=[("rl", b)])
                        if h == 0:
                            R.op("dve", lambda e, b=b, w=w, lo_=lo_, hi=hi: e.tensor_scalar(out=ac[:, lo_:hi], in0=rl[b][:, 0:w], scalar1=wiS[:, qb, 0:1],
                                                                                         scalar2=None, op0=ALU.mult),
                                 reads=[("rl", b), ("wiS", qb)], writes=[ak])
                        else:
                            R.op("dve", lambda e, b=b, w=w, lo_=lo_, hi=hi, h=h: e.scalar_tensor_tensor(
                                out=ac[:, lo_:hi], in0=rl[b][:, 0:w], scalar=wiS[:, qb, h:h + 1], in1=ac[:, lo_:hi], op0=ALU.mult, op1=ALU.add),
                                reads=[("rl", b), ("wiS", qb), ak], writes=[ak])
                R.op("dve", lambda e: e.tensor_tensor(out=ac[:, n - 128:n], in0=ac[:, n - 128:n], in1=g.NTRI[:], op=ALU.add), reads=[ak], writes=[ak])

            def bisect_pair(qa, hooks=()):
                hooks = list(hooks)
                blocks = [qa, qa + 1]
                if qa >= 2:
                    for x_, qb in enumerate(blocks):
                        n = 128 * (qb + 1)
                        ac, ak = acc[x_], ("acc", x_)
                        R.op("dve", lambda e, ac=ac, n=n, x_=x_: e.tensor_reduce(out=bs[:, x_, 0:1], in_=ac[:, 0:n - 128], axis=AX.X, op=ALU.min),
                             reads=[ak], writes=[("lo", x_)])
                        R.op("dve", lambda e, ac=ac, n=n, x_=x_: e.tensor_reduce(out=bs[:, x_, 1:2], in_=ac[:, 0:n], axis=AX.X, op=ALU.max),
                             reads=[ak], writes=[("w0", x_)])
                    for x_ in range(2):
                        R.op("dve", lambda e, x_=x_: e.tensor_tensor(out=bs[:, x_, 1:2], in0=bs[:, x_, 1:2], in1=bs[:, x_, 0:1], op=ALU.subtract),
                             reads=[("w0", x_), ("lo", x_)], writes=[("w0", x_)])
                    for x_ in range(2):
                        R.op("dve", lambda e, x_=x_: e.tensor_scalar(out=wk[:, x_, :], in0=P2[:], scalar1=bs[:, x_, 1:2], scalar2=None, op0=ALU.mult),
                             reads=[("w0", x_), "P2"], writes=[("wk", x_)])
                    for k in range(KBIS):
                        if hooks and k == ((KBIS // 3) if len(hooks) == 2 else (KBIS - 1)):
                            hooks.pop(0)()
                        R.op("dve", lambda e, k=k: e.tensor_tensor(out=bs[:, 0, 2:3], in0=wk[:, 0, k:k + 1], in1=bs[:, 0, 0:1], op=ALU.add),
                             reads=[("wk", 0), ("lo", 0)], writes=[("mid", 0)])
                        R.op("dve", lambda e, k=k: e.scalar_tensor_tensor(out=bs[:, 1, 2:3], in0=wk[:, 1, k:k + 1], scalar=-1.0, in1=bs[:, 1, 0:1],
                                                                          op0=ALU.mult, op1=ALU.subtract), reads=[("wk", 1), ("lo", 1)], writes=[("mid", 1)])
                        n0, n1 = 128 * (blocks[0] + 1), 128 * (blocks[1] + 1)
                        R.op("act", lambda e, n1=n1: e.activation(out=junk[:, 0:n1], in_=acc[1][:, 0:n1], func=AF.Sign, bias=bs[:, 1, 2:3], scale=1.0,
                                                                  accum_out=bs[:, 1, 3:4]), reads=[("acc", 1), ("mid", 1)], writes=[("cnt", 1), "junkA"])
                        R.op("dve", lambda e, n0=n0: e.tensor_scalar(out=maskQ[0][:, 0:n0], in0=acc[0][:, 0:n0], scalar1=bs[:, 0, 2:3], scalar2=None,
                                                                     op0=ALU.is_ge, op1=ALU.add, accum_out=bs[:, 0, 3:4]),
                             reads=[("acc", 0), ("mid", 0)], writes=[("cnt", 0), ("mq", 0)])
                        R.op("dve", lambda e, k=k: e.scalar_tensor_tensor(out=bs[:, 0, 4:5], in0=bs[:, 0, 3:4], scalar=255.5, in1=wk[:, 0, k:k + 1],
                                                                          op0=ALU.is_ge, op1=ALU.mult), reads=[("cnt", 0), ("wk", 0)], writes=[("sel", 0)])
                        R.op("dve", lambda e, k=k, n1=n1: e.scalar_tensor_tensor(out=bs[:, 1, 4:5], in0=bs[:, 1, 3:4], scalar=511.0 - n1, in1=wk[:, 1, k:k + 1],
                                                                                 op0=ALU.is_ge, op1=ALU.mult), reads=[("cnt", 1), ("wk", 1)], writes=[("sel", 1)])
                        for x_ in range(2):
                            R.op("dve", lambda e, x_=x_: e.tensor_tensor(out=bs[:, x_, 0:1], in0=bs[:, x_, 0:1], in1=bs[:, x_, 4:5], op=ALU.add),
                                 reads=[("lo", x_), ("sel", x_)], writes=[("lo", x_)])
                    for x_, qb in enumerate(blocks):
                        n = 128 * (qb + 1)
                        R.op("dve", lambda e, x_=x_, n=n: e.tensor_scalar(out=maskQ[x_][:, 0:n], in0=acc[x_][:, 0:n], scalar1=bs[:, x_, 0:1], scalar2=None, op0=ALU.is_ge),
                             reads=[("acc", x_), ("lo", x_)], writes=[("mq", x_)])
                else:
                    for x_, qb in enumerate(blocks):
                        n = 128 * (qb + 1)
                        R.op("dve", lambda e, x_=x_, n=n: e.tensor_scalar(out=maskQ[x_][:, 0:n], in0=acc[x_][:, 0:n], scalar1=NEG, scalar2=None, op0=ALU.is_gt),
                             reads=[("acc", x_)], writes=[("mq", x_)])
                while hooks:
                    hooks.pop(0)()

            def attend_main(qb):
                mq = maskQ[qb % 2]
                for j0 in range(0, qb + 1, 8):
                    j1 = min(qb + 1, j0 + 8)
                    for j in range(j0, j1):
                        R.op("pe", lambda e, j=j, j0=j0: e.transpose(out=pmT[:, j - j0, :], in_=mq[:, j * 128:(j + 1) * 128], identity=ident[:]),
                             reads=[("mq", qb % 2)], writes=["pmT"])
                    R.op("act", lambda e, j0=j0, j1=j1: e.activation(out=maskT[:, j0:j1, :], in_=pmT[:, 0:j1 - j0, :], func=AF.Copy),
                         reads=["pmT"], writes=["maskT"])
                steps = [(n_, j) for n_ in range(2) for j in range(qb + 1)]
                NB = len(pS)
                LOOK = NB - 1

                def qk(s_):
                    n_, j = steps[s_]
                    b = cnt["pS"] % NB
                    cnt["pS"] += 1
                    R.op("pe", lambda e: e.matmul(pS[b][:], lhsT=kT[:, n_, j * 128:(j + 1) * 128], rhs=qT[:, qb, :, :],
                                                  start=True, stop=True), reads=[("AkT", j), ("AqT", qb)], writes=[("pS", b)])
                    R.op("act", lambda e: e.activation(out=E[b][:], in_=pS[b][:], func=AF.Exp, scale=0.125), reads=[("pS", b)], writes=[("E", b)])
                    R.op("pool", lambda e: e.tensor_tensor(
                        out=E[b][:].rearrange("p (h q) -> p h q", h=4), in0=E[b][:].rearrange("p (h q) -> p h q", h=4),
                        in1=maskT[:, j, :].unsqueeze(1).to_broadcast([128, 4, 128]), op=ALU.mult),
                        reads=[("E", b), "maskT"], writes=[("E", b)])
                    return b

                pend = {}
                for s_ in range(len(steps) + LOOK):
                    if s_ < len(steps):
                        pend[s_] = qk(s_)
                    if s_ - LOOK >= 0:
                        n_, j = steps[s_ - LOOK]
                        b = pend.pop(s_ - LOOK)
                        R.op("pe", lambda e, b=b, j=j, n_=n_: e.matmul(pO[n_][:], lhsT=vaug[:, j, n_:n_ + 2, :], rhs=E[b][:], start=(j == 0), stop=(j == qb)),
                             reads=[("E", b), ("vaug", j), "vones"], writes=[("pO", n_)])

            def attend_dve(qb):
                mx = mixA[qb % 2]
                for n_ in range(2):
                    bo = n_
                    orow = 64 * n_
                    drow = 64 - orow
                    R.op("dve", lambda e, bo=bo, drow=drow: e.reciprocal(out=rden[drow:drow + 64, :], in_=pO[bo][drow:drow + 64, :]), reads=[("pO", bo)], writes=["rden"])
                    for base in range(2):
                        R.op("dve", lambda e, bo=bo, base=base, n_=n_, orow=orow, drow=drow: e.tensor_tensor(
                            out=mx[base * 64:(base + 1) * 64, 2 * n_:2 * n_ + 2, :],
                            in0=pO[bo][orow:orow + 64, :].rearrange("p (a b q) -> p a b q", a=2, b=2)[:, :, base, :],
                            in1=rden[drow:drow + 64, :].rearrange("p (a b q) -> p a b q", a=2, b=2)[:, :, base, :], op=ALU.mult),
                            reads=[("pO", bo), "rden"], writes=[("mixA", qb % 2)])
                outproj_add(g, lambda k: mx[:, k, :], 4, WoA, "WoA", qb, pY, [("mixA", qb % 2)], pykey="pl")

            idx_scores(0)
            idx_scores(1)
            bisect_pair(0)
            for p in range(NT // 2):
                qa = 2 * p
                if p + 1 < NT // 2:
                    idx_scores(qa + 2)
                    idx_scores(qa + 3)
                attend_main(qa)
                h1 = lambda qa=qa: (attend_dve(qa), attend_main(qa + 1))
                h2 = lambda qa=qa: attend_dve(qa + 1)
                if p + 1 < NT // 2:
                    bisect_pair(qa + 2, hooks=(h1, h2))
                else:
                    h1()
                    h2()
            R.emit_phase()


def phase_B(g, i):
    R = g.R
    HT, X, ident = g.HT, g.X, g.ident
    w_in = g.W['ev_w_in'][i].rearrange("(c p) n -> p c n", p=128)
    w_out = g.W['ev_w_out'][i]
    with ExitStack() as st:
        WB = [R.sb("B_W%d" % b, [128, 8, 384], BF16, st) for b in range(2)]
        WoB = [R.sb("B_Wo%d" % b, [128, 1, D], BF16, st) for b in range(2)]
        qkT = [R.sb("B_qkT%d" % b, [128, 3, S], BF16, st) for b in range(2)]
        vaug = [R.sb("B_vaug%d" % b, [128, NT, 2, 128], BF16, st) for b in range(2)]
        mixp = [R.sb("B_mix%d" % b, [128, S], BF16, st) for b in range(2)]
        stg = [R.sb("B_st%d" % b, [128, 256], BF16, st) for b in range(2)]
        tmp = [R.sb("B_rt%d" % b, [128, 128], F32, st) for b in range(2)]
        E = [R.sb("B_E%d" % b, [128, 512], BF16, st) for b in range(3)]
        P = [R.sb("B_P%d" % b, [128, 512], BF16, st) for b in range(3)]
        rden = R.sb("B_rden", [128, 512], F32, st)
        pp = [R.ps("B_pp", [128, 512], F32, st)] * 2
        ptr = R.ps("B_ptr", [128, 8, 128], BF16, st)
        pS = [R.ps("B_pS%d" % b, [128, 512], F32, st) for b in range(3)]
        pO = [R.ps("B_pO%d" % b, [128, 512], F32, st) for b in range(2)]
        pY = [R.ps("B_pY", [128, 512], F32, st)]
        for b in range(2):
            R.op("dve", lambda e, b=b: e.memset(vaug[b][:, :, :, 64:128], 1.0), writes=[("vones", b)])
            R.op("pool", lambda e, b=b: e.memset(qkT[b][:, 0:2, :], 0.0), writes=[("qzero", b)])

        ppS = [R.sb("B_ppS%d" % b, [128, 384], F32, st) for b in range(2)]

        def issue_w(p):
            pb = p % 2
            for k3, off in enumerate((1064, 1576, 2088)):
                wdma(g, WB[pb][:, :, k3 * 128:(k3 + 1) * 128], w_in[:, :, off + p * 128:off + (p + 1) * 128], "wb%d_%d" % (pb, k3), ("WB", pb, k3))

        def issue_wo(p):
            pb = p % 2
            wdma(g, WoB[pb][:, 0, :], w_out[512 + p * 128:512 + (p + 1) * 128, :], "wob%d" % pb, ("WoB", pb))

        def proj(p):
            pb = p % 2
            for t in range(NT):
                b = t % 2
                tok = slice(t * 128, (t + 1) * 128)
                for c in range(8):
                    R.op("pe", lambda e, c=c, b=b, tok=tok: e.matmul(pp[b][:, 0:384], lhsT=HT[:, c, tok], rhs=WB[pb][:, c, :], start=(c == 0), stop=(c == 7)),
                         reads=[("HT", t)] + [("WB", pb, k3) for k3 in range(3)], writes=[("pp", 0)])
                    yield
                R.op("act", lambda e, b=b: e.activation(out=ppS[b][:], in_=pp[b][:, 0:384], func=AF.Copy), reads=[("pp", 0)], writes=[("ppS", b)])
                R.op("pool", lambda e, b=b: e.tensor_copy(out=stg[b][:], in_=ppS[b][:, 0:256]), reads=[("ppS", b)], writes=[("stg", b)])
                rope_tok(g, ppS[b][:, 0:256].rearrange("p (h d) -> p h d", h=4), stg[b][:].rearrange("p (h d) -> p h d", h=4),
                         4, 0, 16, t, tmp, [("ppS", b)], [("stg", b)])
                R.op("pool", lambda e, b=b, t=t: e.tensor_copy(out=vaug[pb][:, t, :, 0:64], in_=ppS[b][:, 256:384].rearrange("p (h d) -> p h d", h=2)),
                     reads=[("ppS", b)], writes=[("Bv", pb)])
                yield
                if t > 0:
                    xpose(pb, t - 1)
                    yield
            xpose(pb, NT - 1)
            yield

        def xpose(pb, t):
            b = t % 2
            tok = slice(t * 128, (t + 1) * 128)
            for x_ in range(2):
                R.op("pe", lambda e, b=b, x_=x_: e.transpose(out=ptr[:, x_, :], in_=stg[b][:, x_ * 128:(x_ + 1) * 128], identity=ident[:]),
                     reads=[("stg", b)], writes=["Bptr"])
            R.op("act", lambda e, tok=tok: e.activation(out=qkT[pb][0:64, 0, tok], in_=ptr[0:64, 0, :], func=AF.Copy), reads=["Bptr", ("qzero", pb)], writes=[("BqkT", pb)])
            R.op("act", lambda e, tok=tok: e.activation(out=qkT[pb][64:128, 1, tok], in_=ptr[64:128, 0, :], func=AF.Copy), reads=["Bptr", ("qzero", pb)], writes=[("BqkT", pb)])
            R.op("act", lambda e, tok=tok: e.activation(out=qkT[pb][:, 2, tok], in_=ptr[:, 1, :], func=AF.Copy), reads=["Bptr"], writes=[("BqkT", pb)])

        def outproj_gen(p, tiles=range(NT)):
            pb = p % 2
            for t in tiles:
                c = t // 4
                outproj_add(g, lambda k, t=t: mixp[pb][:, t * 128:(t + 1) * 128], 1, WoB[pb], ("WoB", pb), t, pY,
                            [("Bmix%d" % pb, c, 0), ("Bmix%d" % pb, c, 1)])
                yield

        def attn(p, filler=None):
            pb = p % 2
            own = (lambda c: outproj_gen(p, range(4 * c, 4 * c + 4))) if p == 3 else None
            attn_pair(g, "B", lambda hh, q0, N: qkT[pb][:, hh, q0:q0 + N],
                      lambda hh, j: qkT[pb][:, 2, j * 128:(j + 1) * 128],
                      lambda hh, j: vaug[pb][:, j, hh, :],
                      [("BqkT", pb), ("Bv", pb), ("vones", pb)], 0.125, "B", mixp[pb], "Bmix%d" % pb, pS, pO, E, P, rden, None,
                      mask_eng=("dve",), filler=filler, own_outproj=own)

        issue_w(0)
        issue_w(1)
        issue_wo(0)
        for _ in proj(0):
            pass
        og = iter(())
        for p in range(4):
            gen = proj(p + 1) if p + 1 < 4 else iter(())
            if p + 2 < 4:
                issue_w(p + 2)
            st_ = {"i": 0}

            def filler(gen=gen, og=og, st_=st_):
                st_["i"] += 1
                next(gen, None)
                if st_["i"] % 8 == 0:
                    next(og, None)

            attn(p, filler=filler)
            for _ in gen:
                pass
            for _ in og:
                pass
            if p + 1 < 4:
                issue_wo(p + 1)
            og = outproj_gen(p) if p < 3 else iter(())
        R.emit_phase()


def ffn_core(g, st, wg, wu, wd, gate_fn, bufs):
    R = g.R
    HT, X = g.HT, g.X
    Wg, Wu, Wd, AT, sg, pG, pU, pY = bufs
    wgv = wg.rearrange("(c p) n -> p c n", p=128)
    wuv = wu.rearrange("(c p) n -> p c n", p=128)
    wdv = wd.rearrange("(c p) n -> p c n", p=128)
    cnt = g.ffn_cnt
    for f in range(NSLAB):
        sb_ = cnt["slab"] % 2
        cnt["slab"] += 1
        wdma(g, Wg[sb_][:], wgv[:, :, f * 512:(f + 1) * 512], "wg%d" % sb_, ("Wg", sb_))
        wdma(g, Wu[sb_][:], wuv[:, :, f * 512:(f + 1) * 512], "wu%d" % sb_, ("Wu", sb_))
        wdma(g, Wd[sb_][:], wdv[:, f * 4:(f + 1) * 4, :], "wd%d" % sb_, ("Wd", sb_))
        for tg in range(4):
            toks = slice(tg * 512, (tg + 1) * 512)
            hkeys = [("HT", t) for t in range(4 * tg, 4 * tg + 4)]
            for ch in range(4):
                b = cnt["gu"] % 2
                cnt["gu"] += 1
                for (pt, Wt, wk, nm) in ((pG, Wg, "Wg", "pG"), (pU, Wu, "Wu", "pU")):
                    for c in range(8):
                        R.op("pe", lambda e, pt=pt, Wt=Wt, c=c, b=b, ch=ch, toks=toks, sb_=sb_: e.matmul(
                            pt[b][:], lhsT=Wt[sb_][:, c, ch * 128:(ch + 1) * 128], rhs=HT[:, c, toks], start=(c == 0), stop=(c == 7)),
                            reads=hkeys + [(wk, sb_)], writes=[(nm, b)])
                R.op("act", lambda e, b=b: e.activation(out=sg[b][:], in_=pG[b][:], func=AF.Silu), reads=[("pG", b)], writes=[("sg", b)])
                R.op("dve", lambda e, b=b, ch=ch, toks=toks: e.tensor_tensor(out=AT[:, ch, toks], in0=pU[b][:], in1=sg[b][:], op=ALU.mult),
                     reads=[("pU", b), ("sg", b)], writes=[("AT", tg)])
        for t in range(NT):
            tok = slice(t * 128, (t + 1) * 128)
            for half in range(2):
                b = cnt["y"] % 2
                cnt["y"] += 1
                hs = slice(half * 512, (half + 1) * 512)
                for ch in range(4):
                    R.op("pe", lambda e, b=b, ch=ch, tok=tok, hs=hs, sb_=sb_: e.matmul(pY[b][:], lhsT=AT[:, ch, tok], rhs=Wd[sb_][:, ch, hs],
                                                                                start=(ch == 0), stop=(ch == 3)),
                         reads=[("AT", t // 4), ("Wd", sb_)], writes=[("pY", b)])
                if gate_fn is None:
                    R.op("dve", lambda e, b=b, t=t, hs=hs: e.tensor_tensor(out=X[:, t, hs], in0=pY[b][:], in1=X[:, t, hs], op=ALU.add),
                         reads=[("pY", b), ("X", t, half)], writes=[("X", t, half)])
                else:
                    R.op("dve", lambda e, b=b, t=t, hs=hs: e.scalar_tensor_tensor(out=X[:, t, hs], in0=pY[b][:], scalar=gate_fn(t), in1=X[:, t, hs],
                                                                                 op0=ALU.mult, op1=ALU.add),
                         reads=[("pY", b), ("X", t, half), "gates"], writes=[("X", t, half)])


def ffn_bufs(g, st):
    R = g.R
    Wg = [R.sb("F_Wg%d" % b, [128, 8, 512], BF16, st) for b in range(2)]
    Wu = [R.sb("F_Wu%d" % b, [128, 8, 512], BF16, st) for b in range(2)]
    Wd = [R.sb("F_Wd%d" % b, [128, 4, D], BF16, st) for b in range(2)]
    AT = R.sb("F_AT", [128, 4, S], BF16, st)
    sg = [R.sb("F_sg%d" % b, [128, 512], BF16, st) for b in range(2)]
    pG = [R.ps("F_pG%d" % b, [128, 512], F32, st) for b in range(2)]
    pU = [R.ps("F_pU%d" % b, [128, 512], F32, st) for b in range(2)]
    pY = [R.ps("F_pY%d" % b, [128, 512], F32, st) for b in range(2)]
    g.ffn_cnt = {"slab": 0, "gu": 0, "y": 0}
    return (Wg, Wu, Wd, AT, sg, pG, pU, pY)


def phase_ffn(g, wg, wu, wd, gate_fn, norm_gain=None, tail=None):
    with ExitStack() as st:
        bufs = ffn_bufs(g, st)
        if norm_gain is not None:
            phase_norm(g, norm_gain, st=st)
        ffn_core(g, st, wg, wu, wd, gate_fn, bufs)
        if tail is not None:
            tail(st)
        g.R.emit_phase()


def phase_moe(g, i, norm_gain=None, tail=None, ntail=None):
    R = g.R
    HT = g.HT
    with ExitStack() as st:
        if norm_gain is not None:
            phase_norm(g, norm_gain, st=st)
        WR = R.sb("M_WR", [128, 8, 8], BF16, st)
        lg = R.sb("M_lg", [128, NT, 8], F32, st)
        gates = R.sb("M_gates", [128, NT, 8], F32, st)
        m8 = R.sb("M_m8", [128, NT, 8], F32, st)
        s12 = R.sb("M_s12", [128, NT], F32, st)
        z = R.sb("M_z", [128, NT, 8], F32, st)
        msk = R.sb("M_msk", [128, NT, 8], F32, st)
        bufs = ffn_bufs(g, st)
        pR = bufs[5][0]
        wdma(g, WR[:], g.W['moe_router'][i].rearrange("(c p) e -> p c e", p=128), "wr", "WR")
        for t in range(NT):
            tok = slice(t * 128, (t + 1) * 128)
            for c in range(8):
                R.op("pe", lambda e, c=c, tok=tok, t=t: e.matmul(pR[:, t * 8:(t + 1) * 8], lhsT=HT[:, c, tok], rhs=WR[:, c, :], start=(c == 0), stop=(c == 7)),
                     reads=[("HT", t), "WR"], writes=[("pG", 0)])
        R.op("act", lambda e: e.activation(out=lg[:].rearrange("p t e -> p (t e)"), in_=pR[:, 0:NT * 8], func=AF.Copy), reads=[("pG", 0)], writes=["lg"])
        for t in range(NT):
            R.op("dve", lambda e, t=t: e.max(out=m8[:, t, :], in_=lg[:, t, :]), reads=["lg"], writes=["m8"])
        R.op("dve", lambda e: e.tensor_tensor(out=s12[:], in0=m8[:, :, 0], in1=m8[:, :, 1], op=ALU.add), reads=["m8"], writes=["s12"])
        R.op("dve", lambda e: e.scalar_tensor_tensor(out=z[:], in0=lg[:], scalar=2.0, in1=s12[:].unsqueeze(2).to_broadcast([128, NT, 8]),
                                                     op0=ALU.mult, op1=ALU.subtract), reads=["lg", "s12"], writes=["z"])
        R.op("act", lambda e: e.activation(out=z[:], in_=z[:], func=AF.Sigmoid), reads=["z"], writes=["z"])
        R.op("dve", lambda e: e.tensor_tensor(out=msk[:], in0=lg[:], in1=m8[:, :, 1:2].to_broadcast([128, NT, 8]), op=ALU.is_ge),
             reads=["lg", "m8"], writes=["msk"])
        R.op("dve", lambda e: e.tensor_tensor(out=gates[:], in0=z[:], in1=msk[:], op=ALU.mult), reads=["z", "msk"], writes=["gates"])
        for ex in range(g.n_experts):
            ffn_core(g, st, g.W['moe_w_gate'][i, ex], g.W['moe_w_up'][i, ex], g.W['moe_w_down'][i, ex],
                     lambda t, ex=ex: gates[:, t, ex:ex + 1], bufs)
        if tail is not None:
            tail(st)
        if ntail is not None:
            ntail(st)
        R.emit_phase(last=(tail is not None))


def phase_final(g, do_final, st=None):
    R = g.R
    X, gbc, ss, rstd = g.X, g.gbc, g.ss, g.rstd
    outv = g.out.rearrange("(t p) d -> p t d", p=128)
    ext = st
    with ExitStack() as st_:
        st = st_ if ext is None else ext
        if do_final:
            junk = R.sb("fjunk", [128, D], BF16, st)
            ob = [R.sb("fob%d" % b, [128, D], F32, st) for b in range(2)]
            R.dma("sp", lambda e: e.dma_start(out=gbc[:], in_=g.W['final_norm'][0:1, :].to_broadcast([128, D])), "gbc", writes=["gbc"])
            for t in range(NT):
                R.op("act", lambda e, t=t: e.activation(out=junk[:], in_=X[:, t, :], func=AF.Square, accum_out=ss[:, t:t + 1]),
                     reads=[("X", t, 0), ("X", t, 1)], writes=["junk", ("ss", t)])
            allss = [("ss", t) for t in range(NT)]
            R.op("dve", lambda e: e.tensor_scalar(out=rstd[:], in0=ss[:], scalar1=1.0 / D, scalar2=EPS, op0=ALU.mult, op1=ALU.add),
                 reads=allss, writes=["rstd"])
            R.op("act", lambda e: e.activation(out=rstd[:], in_=rstd[:], func=AF.Sqrt), reads=["rstd"], writes=["rstd"])
            R.op("dve", lambda e: e.reciprocal(out=rstd[:], in_=rstd[:]), reads=["rstd"], writes=["rstd"])
            for t in range(NT):
                b = t % 2
                R.op("dve", lambda e, t=t, b=b: e.scalar_tensor_tensor(out=ob[b][:], in0=X[:, t, :], scalar=rstd[:, t:t + 1], in1=gbc[:],
                                                                       op0=ALU.mult, op1=ALU.mult),
                     reads=[("X", t, 0), ("X", t, 1), "rstd", "gbc"], writes=[("ob", b)])
                R.dma("sp", lambda e, t=t, b=b: e.dma_start(out=outv[:, t, :], in_=ob[b][:]), "out%d" % b, reads=[("ob", b)], final=True)
        else:
            for t in range(NT):
                R.dma("sp", lambda e, t=t: e.dma_start(out=outv[:, t, :], in_=X[:, t, :]), "out%d" % (t % 2),
                      reads=[("X", t, 0), ("X", t, 1)], final=True)
        if ext is None:
            R.emit_phase(last=True)


def phase_C(g, i, norm_gain=None):
    R = g.R
    HT, X, ident = g.HT, g.X, g.ident
    w_in = g.W['od_w_in'][i].rearrange("(c p) n -> p c n", p=128)
    w_out = g.W['od_w_out'][i]
    w_uq = g.W['mla_w_uq'][i].rearrange("(c p) n -> p c n", p=128)
    w_ukv = g.W['mla_w_ukv'][i]
    scale = 96.0 ** -0.5
    with ExitStack() as st:
        cT = R.sb("C_cT", [128, 3, S], BF16, st)
        krS = R.sb("C_krS", [128, NT, 32], BF16, st)
        with ExitStack() as st2:
            WC = R.sb("C_W", [128, 8, 416], BF16, st2)
            gq = R.sb("C_gq", [128, 256], F32, st2)
            gkv = R.sb("C_gkv", [128, 128], F32, st2)
            junk = R.sb("C_junk", [128, 256], BF16, st2)
            ssq = [R.sb("C_ssq%d" % b, [128, 2], F32, st2) for b in range(2)]
            stc = [R.sb("C_stc%d" % b, [128, 384], BF16, st2) for b in range(2)]
            tmp = [R.sb("C_rt%d" % b, [128, 128], F32, st2) for b in range(2)]
            pc = [R.ps("C_pc%d" % b, [128, 512], F32, st2) for b in range(2)]
            ptr = [R.ps("C_ptr%d" % b, [128, 8, 128], BF16, st2) for b in range(2)]
            wdma(g, WC[:], w_in[:, :, 0:416], "wc", "WC")
            if norm_gain is not None:
                phase_norm(g, norm_gain, st=st2)
            R.dma("sp", lambda e: e.dma_start(out=gq[:], in_=g.W['mla_q_norm'][i:i + 1, :].to_broadcast([128, 256])), "gq", writes=["gq"])
            R.dma("sp", lambda e: e.dma_start(out=gkv[:], in_=g.W['mla_kv_norm'][i:i + 1, :].to_broadcast([128, 128])), "gkv", writes=["gkv"])
            for t in range(NT):
                b = t % 2
                tok = slice(t * 128, (t + 1) * 128)
                for c in range(8):
                    R.op("pe", lambda e, c=c, b=b, tok=tok: e.matmul(pc[b][:, 0:416], lhsT=HT[:, c, tok], rhs=WC[:, c, :], start=(c == 0), stop=(c == 7)),
                         reads=[("HT", t), "WC"], writes=[("pc", b)])
                R.op("act", lambda e, b=b: e.activation(out=junk[:, 0:256], in_=pc[b][:, 0:256], func=AF.Square, accum_out=ssq[b][:, 0:1]),
                     reads=[("pc", b)], writes=["junk", ("ssq", b, 0)])
                R.op("act", lambda e, b=b: e.activation(out=junk[:, 0:128], in_=pc[b][:, 256:384], func=AF.Square, accum_out=ssq[b][:, 1:2]),
                     reads=[("pc", b)], writes=["junk", ("ssq", b, 1)])
                R.op("dve", lambda e, b=b: e.tensor_scalar(out=ssq[b][:, 0:1], in0=ssq[b][:, 0:1], scalar1=1.0 / 256, scalar2=EPS, op0=ALU.mult, op1=ALU.add),
                     reads=[("ssq", b, 0)], writes=[("ssq", b, 0)])
                R.op("dve", lambda e, b=b: e.tensor_scalar(out=ssq[b][:, 1:2], in0=ssq[b][:, 1:2], scalar1=1.0 / 128, scalar2=EPS, op0=ALU.mult, op1=ALU.add),
                     reads=[("ssq", b, 1)], writes=[("ssq", b, 1)])
                R.op("act", lambda e, b=b: e.activation(out=ssq[b][:], in_=ssq[b][:], func=AF.Sqrt), reads=[("ssq", b, 0), ("ssq", b, 1)], writes=[("ssq", b, 2)])
                R.op("dve", lambda e, b=b: e.reciprocal(out=ssq[b][:], in_=ssq[b][:]), reads=[("ssq", b, 2)], writes=[("ssq", b, 3)])
                R.op("dve", lambda e, b=b: e.scalar_tensor_tensor(out=stc[b][:, 0:256], in0=pc[b][:, 0:256], scalar=ssq[b][:, 0:1], in1=gq[:],
                                                                  op0=ALU.mult, op1=ALU.mult), reads=[("pc", b), ("ssq", b, 3), "gq"], writes=[("stc", b)])
                R.op("dve", lambda e, b=b: e.scalar_tensor_tensor(out=stc[b][:, 256:384], in0=pc[b][:, 256:384], scalar=ssq[b][:, 1:2], in1=gkv[:],
                                                                  op0=ALU.mult, op1=ALU.mult), reads=[("pc", b), ("ssq", b, 3), "gkv"], writes=[("stc", b)])
                rope_tok(g, pc[b][:, 384:416].rearrange("p (h d) -> p h d", h=1), krS[:, t, :].rearrange("p (h d) -> p h d", h=1),
                         1, 0, 32, t, tmp, [("pc", b)], [("krS", t)])
                for x_ in range(3):
                    R.op("pe", lambda e, b=b, x_=x_: e.transpose(out=ptr[b][:, x_, :], in_=stc[b][:, x_ * 128:(x_ + 1) * 128], identity=ident[:]),
                         reads=[("stc", b)], writes=[("Cptr", b)])
                R.op("act", lambda e, b=b, tok=tok: e.activation(out=cT[:, :, tok], in_=ptr[b][:, 0:3, :], func=AF.Copy), reads=[("Cptr", b)], writes=[("cT", t)])
            R.emit_phase()
        with ExitStack() as st2:
            Wuq = [R.sb("C_Wuq%d" % b, [128, 2, 192], BF16, st2) for b in range(2)]
            Wukv = [R.sb("C_Wukv%d" % b, [128, 256], BF16, st2) for b in range(2)]
            WoC = [R.sb("C_Wo%d" % b, [128, 1, D], BF16, st2) for b in range(2)]
            qkT = [R.sb("C_qkT%d" % b, [128, 4, S], BF16, st2) for b in range(2)]
            vaug = [R.sb("C_vaug%d" % b, [128, NT, 2, 128], BF16, st2) for b in range(2)]
            mixp = [R.sb("C_mix%d" % b, [128, S], BF16, st2) for b in range(2)]
            ppS = [R.sb("C_ppS%d" % b, [128, 448], F32, st2) for b in range(2)]
            stq = [R.sb("C_stq%d" % b, [128, 192], BF16, st2) for b in range(2)]
            stk = [R.sb("C_stk%d" % b, [128, 2, 96], BF16, st2) for b in range(2)]
            tmp = [R.sb("C_rt2%d" % b, [128, 128], F32, st2) for b in range(2)]
            E = [R.sb("C_E%d" % b, [128, 512], BF16, st2) for b in range(3)]
            P = [R.sb("C_P%d" % b, [128, 512], BF16, st2) for b in range(3)]
            rden = R.sb("C_rden", [128, 512], F32, st2)
            pqkv = R.ps("C_pqkv", [128, 512], F32, st2)
            ptr = R.ps("C_ptr2", [128, 8, 128], BF16, st2)
            pS = [R.ps("C_pS%d" % b, [128, 512], F32, st2) for b in range(3)]
            pO = [R.ps("C_pO%d" % b, [128, 512], F32, st2) for b in range(2)]
            pY = [R.ps("C_pY", [128, 512], F32, st2)]
            for b in range(2):
                R.op("dve", lambda e, b=b: e.memset(vaug[b][:, :, :, 64:128], 1.0), writes=[("vones", b)])
                R.op("pool", lambda e, b=b: e.memset(qkT[b][:], 0.0), writes=[("CqkT", b)])

            def issue_w(p):
                pb = p % 2
                wdma(g, Wuq[pb][:], w_uq[:, :, p * 192:(p + 1) * 192], "wuq%d" % pb, ("Wuq", pb))
                wdma(g, Wukv[pb][:], w_ukv[:, p * 256:(p + 1) * 256], "wukv%d" % pb, ("Wukv", pb))

            def issue_wo(p):
                pb = p % 2
                wdma(g, WoC[pb][:, 0, :], w_out[p * 128:(p + 1) * 128, :], "woc%d" % pb, ("WoC", pb))

            def proj(p):
                pb = p % 2
                for t in range(NT):
                    b = t % 2
                    tok = slice(t * 128, (t + 1) * 128)
                    for c in range(2):
                        R.op("pe", lambda e, c=c, tok=tok: e.matmul(pqkv[:, 0:192], lhsT=cT[:, c, tok], rhs=Wuq[pb][:, c, :], start=(c == 0), stop=(c == 1)),
                             reads=[("cT", t), ("Wuq", pb)], writes=["pq"])
                        yield
                    R.op("pe", lambda e, tok=tok: e.matmul(pqkv[:, 192:448], lhsT=cT[:, 2, tok], rhs=Wukv[pb][:], start=True, stop=True),
                         reads=[("cT", t), ("Wukv", pb)], writes=["pq"])
                    yield
                    R.op("act", lambda e, b=b: e.activation(out=ppS[b][:], in_=pqkv[:, 0:448], func=AF.Copy), reads=["pq"], writes=[("ppS", b)])
                    R.op("pool", lambda e, b=b: e.tensor_copy(out=stq[b][:], in_=ppS[b][:, 0:192]), reads=[("ppS", b)], writes=[("Cstq", b)])
                    rope_tok(g, ppS[b][:, 0:192].rearrange("p (h d) -> p h d", h=2), stq[b][:].rearrange("p (h d) -> p h d", h=2),
                             2, 64, 32, t, tmp, [("ppS", b)], [("Cstq", b)])
                    R.op("pool", lambda e, b=b: e.tensor_copy(out=stk[b][:, :, 0:64], in_=ppS[b][:, 192:448].rearrange("p (h d) -> p h d", h=2)[:, :, 0:64]),
                         reads=[("ppS", b)], writes=[("Cstk", b)])
                    R.op("pool", lambda e, b=b, t=t: e.tensor_copy(out=stk[b][:, :, 64:96], in_=krS[:, t, :].unsqueeze(1).to_broadcast([128, 2, 32])),
                         reads=[("krS", t)], writes=[("Cstk", b)])
                    R.op("pool", lambda e, b=b, t=t: e.tensor_copy(out=vaug[pb][:, t, :, 0:64], in_=ppS[b][:, 192:448].rearrange("p (h d) -> p h d", h=2)[:, :, 64:128]),
                         reads=[("ppS", b)], writes=[("Cv", pb)])
                    yield
                    if t > 0:
                        xpose(pb, t - 1)
                        yield
                xpose(pb, NT - 1)
                yield

            def xpose(pb, t):
                b = t % 2
                tok = slice(t * 128, (t + 1) * 128)
                for hh in range(2):
                    R.op("pe", lambda e, b=b, hh=hh: e.transpose(out=ptr[0:96, hh, :], in_=stq[b][:, hh * 96:(hh + 1) * 96], identity=ident[:]),
                         reads=[("Cstq", b)], writes=["Cptr2"])
                    R.op("pe", lambda e, b=b, hh=hh: e.transpose(out=ptr[0:96, 2 + hh, :], in_=stk[b][:, hh, :], identity=ident[:]),
                         reads=[("Cstk", b)], writes=["Cptr2"])
                R.op("act", lambda e, tok=tok: e.activation(out=qkT[pb][0:96, :, tok], in_=ptr[0:96, 0:4, :], func=AF.Copy), reads=["Cptr2"], writes=[("CqkT", pb)])

            def outproj_gen(p, tiles=range(NT)):
                pb = p % 2
                for t in tiles:
                    c = t // 4
                    outproj_add(g, lambda k, t=t: mixp[pb][:, t * 128:(t + 1) * 128], 1, WoC[pb], ("WoC", pb), t, pY,
                                [("Cmix%d" % pb, c, 0), ("Cmix%d" % pb, c, 1)])
                    yield

            def attn(p, filler=None):
                pb = p % 2
                own = (lambda c: outproj_gen(p, range(4 * c, 4 * c + 4))) if p == 3 else None
                attn_pair(g, "C", lambda hh, q0, N: qkT[pb][0:96, hh, q0:q0 + N],
                          lambda hh, j: qkT[pb][0:96, 2 + hh, j * 128:(j + 1) * 128],
                          lambda hh, j: vaug[pb][:, j, hh, :],
                          [("CqkT", pb), ("Cv", pb), ("vones", pb)], scale, "C", mixp[pb], "Cmix%d" % pb, pS, pO, E, P, rden, None,
                          mask_eng=("dve",), filler=filler, own_outproj=own)

            issue_w(0)
            issue_w(1)
            issue_wo(0)
            for _ in proj(0):
                pass
            og = iter(())
            for p in range(4):
                gen = proj(p + 1) if p + 1 < 4 else iter(())
                if p + 2 < 4:
                    issue_w(p + 2)
                st_ = {"i": 0}

                def filler(gen=gen, og=og, st_=st_):
                    st_["i"] += 1
                    next(gen, None)
                    if st_["i"] % 8 == 0:
                        next(og, None)

                attn(p, filler=filler)
                for _ in gen:
                    pass
                for _ in og:
                    pass
                if p + 1 < 4:
                    issue_wo(p + 1)
                og = outproj_gen(p) if p < 3 else iter(())
            R.emit_phase()


def phase_D(g, i):
    R = g.R
    HT, X = g.HT, g.X
    w_in = g.W['od_w_in'][i].rearrange("(c p) n -> p c n", p=128)
    w_out = g.W['od_w_out'][i]
    with ExitStack() as st:
        WD = R.sb("D_W", [128, 8, 256], BF16, st)
        WoD = R.sb("D_Wo", [128, 1, D], BF16, st)
        Wbd2 = [R.sb("D_Wbd%d" % b, [128, 2, 128], BF16, st) for b in range(2)]
        cols2 = [R.sb("D_cols%d" % b, [128, 12], F32, st) for b in range(2)]
        xrS2 = [R.sb("D_xr%d" % b, [128, S + 4], F32, st) for b in range(2)]
        xc2 = [R.sb("D_xc%d" % b, [128, S], F32, st) for b in range(2)]
        av2 = [R.sb("D_a%d" % b, [128, S], F32, st) for b in range(2)]
        uv2 = [R.sb("D_u%d" % b, [128, S], F32, st) for b in range(2)]
        gl2 = [R.sb("D_gl%d" % b, [128, S], BF16, st) for b in range(2)]
        xcb = R.sb("D_xcb", [128, S], BF16, st)
        mixD = R.sb("D_mix", [128, S], BF16, st)
        rr = R.sb("D_rr", [128, 512], F32, st)
        sq = R.sb("D_sq", [128, 512], F32, st)
        ig = R.sb("D_ig", [128, 512], F32, st)
        pxr = [R.ps("D_pxr%d" % b, [128, 512], F32, st) for b in range(2)]
        pgt = [R.ps("D_pgt%d" % b, [128, 512], F32, st) for b in range(2)]
        pa = R.ps("D_pa", [128, 512], F32, st)
        px = R.ps("D_px", [128, 512], F32, st)
        pY = [R.ps("D_pY", [128, 512], F32, st)]
        for b in range(2):
            R.op("dve", lambda e, b=b: e.memset(xrS2[b][:, 0:3], 0.0), writes=[("xpad", b)])

        def stage_a(ct):
            pb = ct % 2
            Wbd, cols, xrS, gl = Wbd2[pb], cols2[pb], xrS2[pb], gl2[pb]
            ch = slice(ct * 128, (ct + 1) * 128)
            wdma(g, WD[:, :, 0:128], w_in[:, :, 416 + ct * 128:416 + (ct + 1) * 128], "wd0", ("WD", 0))
            wdma(g, WD[:, :, 128:256], w_in[:, :, 928 + ct * 128:928 + (ct + 1) * 128], "wd1", ("WD", 1))
            R.op("pool", lambda e: e.memset(Wbd[:], 0.0), writes=[("Wbd", pb)])
            for s_, nm in enumerate(('rg_w_a', 'rg_w_x')):
                for hb_ in range(2):
                    R.dma("pool", lambda e, s_=s_, nm=nm, hb_=hb_: e.dma_start(
                        out=Wbd[hb_ * 64:(hb_ + 1) * 64, s_, hb_ * 64:(hb_ + 1) * 64], in_=g.W[nm][i, 2 * ct + hb_]),
                        "wbd%d%d%d" % (pb, s_, hb_), reads=[("Wbd", pb)], writes=[("Wbdb", pb, s_, hb_)])
            srcs = [g.W['rg_conv_w'][i, j:j + 1, ch] for j in range(4)] + [g.W[nm][i:i + 1, ch] for nm in ('rg_conv_b', 'rg_b_a', 'rg_b_x', 'rg_lambda')]
            for k_, src in enumerate(srcs):
                R.dma("sp", lambda e, k_=k_, src=src: e.dma_start(out=cols[:, k_:k_ + 1], in_=src.rearrange("o p -> p o")), "dcol%d_%d" % (pb, k_),
                      writes=[("cols", pb, k_)])
            R.op("act", lambda e: e.activation(out=cols[:, 8:9], in_=cols[:, 7:8], func=AF.Exp, scale=-1.0), reads=[("cols", pb, 7)], writes=[("cols", pb, 8)])
            R.op("act", lambda e: e.activation(out=cols[:, 8:9], in_=cols[:, 8:9], func=AF.Ln, bias=1.0), reads=[("cols", pb, 8)], writes=[("cols", pb, 8)])
            R.op("dve", lambda e: e.tensor_scalar(out=cols[:, 9:10], in0=cols[:, 8:9], scalar1=-16.0, scalar2=None, op0=ALU.mult),
                 reads=[("cols", pb, 8)], writes=[("cols", pb, 9)])
            R.op("dve", lambda e: e.tensor_scalar(out=cols[:, 8:9], in0=cols[:, 8:9], scalar1=-8.0, scalar2=None, op0=ALU.mult),
                 reads=[("cols", pb, 8), ("cols", pb, 9)], writes=[("cols", pb, 8)])
            for tg in range(4):
                b = tg % 2
                toks = slice(tg * 512, (tg + 1) * 512)
                hkeys = [("HT", t) for t in range(4 * tg, 4 * tg + 4)]
                for (pt, lo, nm, wk) in ((pxr, 0, "pxr", 0), (pgt, 128, "pgt", 1)):
                    for c in range(8):
                        R.op("pe", lambda e, pt=pt, lo=lo, c=c, b=b, toks=toks: e.matmul(pt[b][:], lhsT=WD[:, c, lo:lo + 128], rhs=HT[:, c, toks],
                                                                                       start=(c == 0), stop=(c == 7)),
                             reads=hkeys + [("WD", wk)], writes=[(nm, b)])
                R.op("act", lambda e, b=b, tg=tg: e.activation(out=xrS[:, 3 + tg * 512:3 + (tg + 1) * 512], in_=pxr[b][:], func=AF.Copy),
                     reads=[("pxr", b)], writes=[("xr", pb, tg)])
                R.op("act", lambda e, b=b, toks=toks: e.activation(out=gl[:, toks], in_=pgt[b][:], func=AF.Gelu_apprx_tanh), reads=[("pgt", b)], writes=[("gl", pb, tg)])

        def stage_b(ct):
            pb = ct % 2
            Wbd, cols, xrS, xc, av, uv = Wbd2[pb], cols2[pb], xrS2[pb], xc2[pb], av2[pb], uv2[pb]
            bdkeys = [("Wbdb", pb, s_, hb_) for s_ in range(2) for hb_ in range(2)]
            xrk = [("xr", pb, tg) for tg in range(4)] + [("xpad", pb)]
            R.op("dve", lambda e: e.tensor_scalar(out=xc[:], in0=xrS[:, 0:S], scalar1=cols[:, 0:1], scalar2=cols[:, 4:5], op0=ALU.mult, op1=ALU.add),
                 reads=xrk + [("cols", pb, 0), ("cols", pb, 4)], writes=[("xc", pb)])
            for j in range(1, 4):
                R.op("dve", lambda e, j=j: e.scalar_tensor_tensor(out=xc[:], in0=xrS[:, j:j + S], scalar=cols[:, j:j + 1], in1=xc[:], op0=ALU.mult, op1=ALU.add),
                     reads=xrk + [("cols", pb, j), ("xc", pb)], writes=[("xc", pb)])
            R.op("act", lambda e: e.activation(out=xcb[:], in_=xc[:], func=AF.Copy), reads=[("xc", pb)], writes=["xcb"])
            for tg in range(4):
                toks = slice(tg * 512, (tg + 1) * 512)
                R.op("pe", lambda e, toks=toks: e.matmul(pa[:], lhsT=Wbd[:, 0, :], rhs=xcb[:, toks], start=True, stop=True), reads=["xcb", ("Wbd", pb)] + bdkeys, writes=["pa"])
                R.op("pe", lambda e, toks=toks: e.matmul(px[:], lhsT=Wbd[:, 1, :], rhs=xcb[:, toks], start=True, stop=True), reads=["xcb", ("Wbd", pb)] + bdkeys, writes=["px"])
                R.op("act", lambda e: e.activation(out=rr[:], in_=pa[:], func=AF.Sigmoid, bias=cols[:, 5:6]), reads=["pa", ("cols", pb, 5)], writes=["rr"])
                R.op("act", lambda e: e.activation(out=ig[:], in_=px[:], func=AF.Sigmoid, bias=cols[:, 6:7]), reads=["px", ("cols", pb, 6)], writes=["ig"])
                R.op("act", lambda e, toks=toks: e.activation(out=av[:, toks], in_=rr[:], func=AF.Exp, scale=cols[:, 8:9]), reads=["rr", ("cols", pb, 8)], writes=[("av", pb, tg)])
                R.op("act", lambda e: e.activation(out=sq[:], in_=rr[:], func=AF.Exp, scale=cols[:, 9:10]), reads=["rr", ("cols", pb, 9)], writes=["sq"])
                R.op("act", lambda e: e.activation(out=sq[:], in_=sq[:], func=AF.Sqrt, scale=-1.0, bias=1.0), reads=["sq"], writes=["sq"])
                R.op("dve", lambda e, toks=toks: e.tensor_tensor(out=uv[:, toks], in0=ig[:], in1=xc[:, toks], op=ALU.mult), reads=["ig", ("xc", pb)], writes=[("uv", pb, tg)])
                R.op("dve", lambda e, toks=toks: e.tensor_tensor(out=uv[:, toks], in0=uv[:, toks], in1=sq[:], op=ALU.mult), reads=[("uv", pb, tg), "sq"], writes=[("uv", pb, tg)])

        def stage_c(ct):
            pb = ct % 2
            xrS, av, uv, gl = xrS2[pb], av2[pb], uv2[pb], gl2[pb]
            xrk = [("xr", pb, tg) for tg in range(4)] + [("xpad", pb)]
            wdma(g, WoD[:, 0, :], w_out[512 + ct * 128:512 + (ct + 1) * 128, :], "wod", "WoD")
            R.op("dve", lambda e: e.tensor_tensor_scan(out=xrS[:, 4:4 + S], data0=av[:], data1=uv[:], initial=0.0, op0=ALU.mult, op1=ALU.add),
                 reads=[("av", pb, tg) for tg in range(4)] + [("uv", pb, tg) for tg in range(4)] + xrk, writes=[("hsc", pb)] + [("xr", pb, tg) for tg in range(4)])
            R.op("dve", lambda e: e.tensor_tensor(out=mixD[:], in0=xrS[:, 4:4 + S], in1=gl[:], op=ALU.mult),
                 reads=[("hsc", pb)] + [("gl", pb, tg) for tg in range(4)], writes=["mixD"])
            for t in range(NT):
                outproj_add(g, lambda k, t=t: mixD[:, t * 128:(t + 1) * 128], 1, WoD, "WoD", t, pY, ["mixD"])

        stage_a(0)
        stage_b(0)
        for ct in range(4):
            if ct + 1 < 4:
                stage_a(ct + 1)
            stage_c(ct)
            if ct + 1 < 4:
                stage_b(ct + 1)
        R.emit_phase()


_NC_CACHE = {}


def _get_nc(key, **kw):
    if key not in _NC_CACHE:
        _NC_CACHE[key] = build(**kw)
    return _NC_CACHE[key]


def _in_maps(inputs, n):
    shared = {}
    for name, shp in IN_SHAPES.items():
        shared[name] = np.ascontiguousarray(np.asarray(inputs[name], dtype=np.float32).reshape(shp))
    x = np.asarray(inputs['x'], dtype=np.float32)
    maps = []
    for b in range(n):
        m = dict(shared)
        m['x'] = np.ascontiguousarray(x[b])
        maps.append(m)
    return maps


def kernel(**inputs):
    from concourse.bass_utils import run_bass_kernel_spmd
    n = 8
    nc = _get_nc("full")
    res = run_bass_kernel_spmd(nc, _in_maps(inputs, n), core_ids=list(range(n)))
    return np.stack([np.asarray(r["out"], dtype=np.float32) for r in res.results], axis=0)
```
